# Optimizing a Trainium2 kernel written in Bass

```python
import math
import jax, jax.numpy as jnp
from jax import lax
import numpy as np

D_MODEL = 1024
BATCH = 16
SEQ = 4096
DEPTH = 1

HEAD_DIM = 64
SB_HEADS = 8
DIL_GROUPS = ((128, 1), (512, 4), (2048, 16))
DIL_HEADS_PER_GROUP = 4
DIL_HEADS = DIL_HEADS_PER_GROUP * len(DIL_GROUPS)
N_BRANCHES = 2
Q_BLOCK = 128
PEER_HEADS = 8
PEER_N_KEYS = 128
PEER_N_EXPERTS = PEER_N_KEYS * PEER_N_KEYS
PEER_QUERY_DIM = 256
PEER_TOPK = 16
TOKEN_CHUNK = 128
RMS_EPS = 1e-6
ALIBI_MAX_EXP = 8.0

SB_W = SB_HEADS * HEAD_DIM
DIL_W = DIL_HEADS * HEAD_DIM
DIL_OUT_W = DIL_HEADS_PER_GROUP * HEAD_DIM
IN_PROJ_W = 3 * SB_W + 3 * DIL_W + N_BRANCHES * D_MODEL

kernel_name = "hybrid_stickbreak_dilated_peer_block"


def rmsnorm(x, gain):
    xf = x.astype(jnp.float32)
    y = xf * lax.rsqrt(jnp.mean(xf * xf, axis=-1, keepdims=True) + RMS_EPS)
    return (y * gain.astype(jnp.float32)).astype(x.dtype)


def stick_breaking_attention(q, k, v):
    B, S, H, dh = q.shape
    nb = S // Q_BLOCK
    scale = dh ** -0.5
    q_blocks = q.reshape(B, nb, Q_BLOCK, H, dh).transpose(1, 0, 2, 3, 4)
    key_pos = jnp.arange(S)

    def block(args):
        qb, bi = args
        q_pos = bi * Q_BLOCK + jnp.arange(Q_BLOCK)
        z = jnp.einsum('bqhd,bshd->bhqs', qb, k).astype(jnp.float32) * scale
        causal = key_pos[None, :] < q_pos[:, None]
        log_beta = jax.nn.log_sigmoid(z)
        log_stay = jnp.where(causal, jax.nn.log_sigmoid(-z), 0.0)
        cum = jnp.cumsum(log_stay, axis=-1)
        log_a = log_beta + (cum[..., -1:] - cum)
        a = jnp.where(causal, jnp.exp(log_a), 0.0)
        return jnp.einsum('bhqs,bshd->bqhd', a.astype(v.dtype), v)

    out = lax.map(block, (q_blocks, jnp.arange(nb)))
    return out.transpose(1, 0, 2, 3, 4).reshape(B, S, H, dh)


def dilated_attention(q, k, v, slopes):
    B, S, H, dh = q.shape
    Hg = DIL_HEADS_PER_GROUP
    nb = S // Q_BLOCK
    scale = dh ** -0.5
    q_blocks = q.reshape(B, nb, Q_BLOCK, H, dh).transpose(1, 0, 2, 3, 4)
    k_groups = [k[:, :, g * Hg:(g + 1) * Hg] for g in range(len(DIL_GROUPS))]
    v_groups = [v[:, :, g * Hg:(g + 1) * Hg] for g in range(len(DIL_GROUPS))]

    def block(args):
        qb, bi = args
        q_pos = bi * Q_BLOCK + jnp.arange(Q_BLOCK)
        outs, lses = [], []
        for g, (window, dil) in enumerate(DIL_GROUPS):
            offs = dil * jnp.arange(window // dil + 1)
            idx = q_pos[:, None] - offs[None, :]
            valid = idx >= 0
            idx = jnp.maximum(idx, 0)
            kg = jnp.take(k_groups[g], idx, axis=1)
            vg = jnp.take(v_groups[g], idx, axis=1)
            qg = qb[:, :, g * Hg:(g + 1) * Hg]
            z = jnp.einsum('bqhd,bqjhd->bhqj', qg, kg).astype(jnp.float32) * scale
            z = z - slopes[g * Hg:(g + 1) * Hg][:, None, None] * offs.astype(jnp.float32)[None, None, :]
            z = jnp.where(valid[None, None], z, -1e30)
            m = jnp.max(z, axis=-1, keepdims=True)
            p = jnp.exp(z - m)
            den = jnp.sum(p, axis=-1, keepdims=True)
            o = jnp.einsum('bhqj,bqjhd->bqhd', (p / den).astype(vg.dtype), vg)
            outs.append(o)
            lses.append((m + jnp.log(den))[..., 0])
        w = jax.nn.softmax(jnp.stack(lses, axis=0), axis=0)
        w = w.transpose(0, 1, 3, 2)[..., None]
        o_all = jnp.stack(outs, axis=0)
        return jnp.sum(w.astype(o_all.dtype) * o_all, axis=0)

    out = lax.map(block, (q_blocks, jnp.arange(nb)))
    return out.transpose(1, 0, 2, 3, 4).reshape(B, S, Hg, dh)


def peer_ffn(h, w_q, sub_keys, expert_u, expert_v):
    B, S, D = h.shape
    t = h.reshape(-1, D)
    T = t.shape[0]
    chunks = t.reshape(T // TOKEN_CHUNK, TOKEN_CHUNK, D)
    K = PEER_TOPK

    def chunk(xc):
        qh = (xc @ w_q).reshape(TOKEN_CHUNK, PEER_HEADS, 2, PEER_QUERY_DIM // 2)
        s = jnp.einsum('thpc,hpnc->thpn', qh, sub_keys).astype(jnp.float32)
        s_top, i_top = lax.top_k(s, K)
        cand = (s_top[:, :, 0, :, None] + s_top[:, :, 1, None, :]).reshape(TOKEN_CHUNK, PEER_HEADS, K * K)
        best, ci = lax.top_k(cand, K)
        i1 = jnp.take_along_axis(i_top[:, :, 0], ci // K, axis=-1)
        i2 = jnp.take_along_axis(i_top[:, :, 1], ci % K, axis=-1)
        e = i1 * PEER_N_KEYS + i2
        gates = jax.nn.softmax(best, axis=-1)
        u = jnp.take(expert_u, e, axis=0)
        vv = jnp.take(expert_v, e, axis=0)
        act = jax.nn.gelu(jnp.einsum('thkd,td->thk', u, xc).astype(jnp.float32), approximate=False)
        return jnp.einsum('thk,thkd->td', (gates * act).astype(vv.dtype), vv)

    out = lax.map(chunk, chunks)
    return out.reshape(B, S, D)


def setup_inputs(seed: int = 0) -> dict:
    key = jax.random.key(seed)
    ks = jax.random.split(key, 14)
    D = D_MODEL
    f32 = jnp.float32
    nrm = lambda k, shape, s: jax.random.normal(k, shape, f32) * s
    return {
        "x": jax.random.normal(ks[0], (BATCH, SEQ, D), f32),
        "norm1_gain": 1.0 + nrm(ks[1], (DEPTH, D), 0.02),
        "w_in": nrm(ks[2], (DEPTH, D, IN_PROJ_W), D ** -0.5),
        "b_gate": nrm(ks[3], (DEPTH, N_BRANCHES * D), 0.02),
        "q_norm_gain": 1.0 + nrm(ks[4], (DEPTH, DIL_HEADS, HEAD_DIM), 0.02),
        "k_norm_gain": 1.0 + nrm(ks[5], (DEPTH, DIL_HEADS, HEAD_DIM), 0.02),
        "w_sb_out": nrm(ks[6], (DEPTH, SB_W, D), SB_W ** -0.5),
        "w_dil_out": nrm(ks[7], (DEPTH, DIL_OUT_W, D), DIL_OUT_W ** -0.5),
        "w_out": nrm(ks[8], (DEPTH, D, D), D ** -0.5),
        "norm2_gain": 1.0 + nrm(ks[9], (DEPTH, D), 0.02),
        "w_peer_q": nrm(ks[10], (DEPTH, D, PEER_HEADS * PEER_QUERY_DIM), D ** -0.5),
        "peer_sub_keys": nrm(ks[11], (DEPTH, PEER_HEADS, 2, PEER_N_KEYS, PEER_QUERY_DIM // 2), (PEER_QUERY_DIM // 2) ** -0.5),
        "peer_u": nrm(ks[12], (DEPTH, PEER_N_EXPERTS, D), D ** -0.5),
        "peer_v": nrm(ks[13], (DEPTH, PEER_N_EXPERTS, D), 0.3),
    }


def reference(x, norm1_gain, w_in, b_gate, q_norm_gain, k_norm_gain, w_sb_out, w_dil_out, w_out,
              norm2_gain, w_peer_q, peer_sub_keys, peer_u, peer_v):
    B, S, D = x.shape
    splits = [SB_W, 2 * SB_W, 3 * SB_W, 3 * SB_W + DIL_W, 3 * SB_W + 2 * DIL_W, 3 * SB_W + 3 * DIL_W]
    slopes = 2.0 ** (-ALIBI_MAX_EXP * jnp.arange(1, DIL_HEADS + 1, dtype=jnp.float32) / DIL_HEADS)
    for l in range(DEPTH):
        xn = rmsnorm(x, norm1_gain[l])
        proj = xn @ w_in[l]
        q_sb, k_sb, v_sb, q_dl, k_dl, v_dl, gate_logits = jnp.split(proj, splits, axis=-1)
        hd = lambda t, h: t.reshape(B, S, h, HEAD_DIM)
        o_sb = stick_breaking_attention(hd(q_sb, SB_HEADS), hd(k_sb, SB_HEADS), hd(v_sb, SB_HEADS))
        q_dl = rmsnorm(hd(q_dl, DIL_HEADS), q_norm_gain[l])
        k_dl = rmsnorm(hd(k_dl, DIL_HEADS), k_norm_gain[l])
        o_dl = dilated_attention(q_dl, k_dl, hd(v_dl, DIL_HEADS), slopes)
        y_sb = o_sb.reshape(B, S, SB_W) @ w_sb_out[l]
        y_dl = o_dl.reshape(B, S, DIL_OUT_W) @ w_dil_out[l]
        gates = jax.nn.sigmoid((gate_logits + b_gate[l]).astype(jnp.float32)).astype(x.dtype)
        g_sb, g_dl = jnp.split(gates, 2, axis=-1)
        x = x + (g_sb * y_sb + g_dl * y_dl) @ w_out[l]
        x = x + peer_ffn(rmsnorm(x, norm2_gain[l]), w_peer_q[l], peer_sub_keys[l], peer_u[l], peer_v[l])
    return x
```

```python
from contextlib import ExitStack
import numpy as np
import concourse.bass as bass
import concourse.mybir as mybir
from concourse.bass_utils import run_bass_kernel_spmd

F32 = mybir.dt.float32
BF16 = mybir.dt.bfloat16
I32 = mybir.dt.int32
U32 = mybir.dt.uint32
AF = mybir.ActivationFunctionType
ALU = mybir.AluOpType
AX = mybir.AxisListType

ENGS = ("sync", "scalar", "vector", "gpsimd", "tensor")
SEM_LIMIT = 24000

S_LEN = 4096
D = 1024
NSEQ = 2
NCORES = 8
EPS = 1e-6
SBW = 512
DLW = 768
QB = 512
DIL = (1, 4, 16)


class Buf:
    __slots__ = ("name", "last_w", "readers", "dsem")

    def __init__(self, name=""):
        self.name = name
        self.last_w = None
        self.readers = []
        self.dsem = None


class Op:
    __slots__ = ("eng", "fn", "deps", "is_dma", "dsem", "sig", "need", "waits")

    def __init__(self, eng, fn, deps, is_dma, dsem):
        self.eng = eng
        self.fn = fn
        self.deps = deps
        self.is_dma = is_dma
        self.dsem = dsem
        self.sig = None
        self.need = False
        self.waits = None


class Sched:
    def __init__(self, nc, es):
        self.nc = nc
        self.es = es
        self.ops = []
        self.nsem = 0
        self.esem = {}
        self.ecnt = {}
        for e in ENGS:
            self.esem[e] = self._newsem("e_" + e)
            self.ecnt[e] = 0
        self.waited = {e: {} for e in ENGS}
        self.dcum = {}
        self.total_ops = 0

    def _newsem(self, name):
        self.nsem += 1
        return self.es.enter_context(self.nc.semaphore(f"{name}_{self.nsem}"))

    def dsem_for(self, buf):
        if buf.dsem is None:
            buf.dsem = self._newsem("d")
            self.dcum[buf.dsem] = 0
        return buf.dsem

    def share_dsem(self, bufs):
        s = self.dsem_for(bufs[0])
        for b in bufs[1:]:
            b.dsem = s

    def _deps(self, reads, writes):
        deps = []
        for b in reads:
            if b.last_w is not None:
                deps.append(b.last_w)
        for b in writes:
            if b.last_w is not None:
                deps.append(b.last_w)
            deps.extend(b.readers)
        return deps

    def _commit(self, op, reads, writes):
        for b in reads:
            b.readers.append(op)
        for b in writes:
            b.last_w = op
            b.readers = []
        self.ops.append(op)

    def op(self, eng, fn, reads=(), writes=()):
        o = Op(eng, fn, self._deps(reads, writes), False, None)
        self._commit(o, reads, writes)
        return o

    def dma(self, eng, fn, reads=(), writes=(), sembuf=None):
        if sembuf is None:
            sembuf = writes[0] if writes else reads[0]
        ds = self.dsem_for(sembuf)
        o = Op(eng, fn, self._deps(reads, writes), True, ds)
        self._commit(o, reads, writes)
        return o

    def emit(self, final_wait_eng="sync"):
        ops = self.ops
        for o in ops:
            for d in o.deps:
                if not (o.eng == "tensor" and d.eng == "tensor"):
                    d.need = True
        per_eng = {e: [] for e in ENGS}
        dma_seen = {}
        for o in ops:
            e = o.eng
            waits = []
            w = self.waited[e]
            for d in o.deps:
                if d.sig is None:
                    continue
                if e == "tensor" and d.eng == "tensor" and not d.is_dma:
                    continue
                s, v = d.sig
                if d.is_dma:
                    v = max(v, dma_seen.get(s, v))
                if w.get(s, 0) < v:
                    w[s] = v
                    waits.append((s, v))
            o.waits = waits
            if o.is_dma:
                self.dcum[o.dsem] += 16
                o.sig = (o.dsem, self.dcum[o.dsem])
                dma_seen[o.dsem] = self.dcum[o.dsem]
            elif o.need:
                if self.ecnt[e] >= SEM_LIMIT:
                    self.esem[e] = self._newsem("e_" + e)
                    self.ecnt[e] = 0
                self.ecnt[e] += 1
                o.sig = (self.esem[e], self.ecnt[e])
            per_eng[e].append(o)
        finals = list(dma_seen.items())
        nc = self.nc
        with nc.Block() as block:
            for e in ENGS:
                lst = per_eng[e]
                if not lst and e != final_wait_eng:
                    continue

                def body(eng, lst=lst, e=e):
                    for o in lst:
                        for (s, v) in o.waits:
                            eng.wait_ge(s, v)
                        ins = o.fn(eng)
                        if o.is_dma:
                            ins.then_inc(o.sig[0], 16)
                        elif o.sig is not None:
                            ins.then_inc(o.sig[0], 1)
                    if e == final_wait_eng:
                        for (s, v) in finals:
                            eng.wait_ge(s, v)

                getattr(block, e)(body)
        self.total_ops += len(ops)
        for o in ops:
            o.sig = None
        self.ops = []


class Ring:
    def __init__(self, tiles):
        self.tiles = tiles
        self.bufs = [Buf() for _ in tiles]
        self.i = 0

    def next(self):
        i = self.i % len(self.tiles)
        self.i += 1
        return self.tiles[i], self.bufs[i]


C_IDENT = 0
C_NEGU = 128
C_NEGONE = 256
C_MBIG = 384
C_ONES = 1280
C_IOTA = 1408
C_BD = 1424
C_END = 1552


def host_consts():
    c = np.zeros((128, C_END), np.float32)
    i = np.arange(128)[:, None]
    j = np.arange(128)[None, :]
    c[:, C_IDENT:C_IDENT + 128] = (i == j)
    c[:, C_NEGU:C_NEGU + 128] = -1.0 * (i >= j)
    c[:, C_NEGONE:C_NEGONE + 128] = -1.0
    cc = np.arange(896)[None, :]
    c[:, C_MBIG:C_MBIG + 896] = ((cc - 384) > i)
    c[:, C_ONES:C_ONES + 128] = 1.0
    c[:, C_IOTA:C_IOTA + 16] = np.arange(16)[None, :]
    c[:, C_BD:C_BD + 128] = ((i // 64) == (j // 64))
    return c


def host_alibi():
    slopes = 2.0 ** (-8.0 * np.arange(1, 13, dtype=np.float64) / 12.0)
    out = np.zeros((6, 128, 1024), np.float32)
    s = np.arange(128)[:, None].astype(np.float64)
    t = np.arange(128)[None, :].astype(np.float64)
    for h in range(12):
        r = DIL[h // 4]
        m = slopes[h] * r
        diag = np.where(t >= s, np.exp(-m * np.maximum(t - s, 0)), 0.0)
        prev = np.where(t <= s, np.exp(-m * (t - s + 128)), 0.0)
        pi, e = h // 2, h % 2
        out[pi, :, e * 256:e * 256 + 128] = prev
        out[pi, :, e * 256 + 128:e * 256 + 256] = diag
        out[pi, :, 512 + e * 256 + 128:512 + e * 256 + 256] = diag
    return out


def build(debug=None, nseq=NSEQ):
    debug = debug or {}
    nc = bass.Bass("TRN2", target_bir_lowering=False)
    dram_in = lambda name, shape, dt=F32: nc.dram_tensor(name, shape, dt, kind="ExternalInput").ap()
    x_d = dram_in("x", [NSEQ, S_LEN, D])
    g1_d = dram_in("g1", [128, D])
    win_d = dram_in("w_in", [D, 5888])
    bg_d = dram_in("b_gate", [128, 16])
    qg_d = dram_in("qg", [128, 6])
    kg_d = dram_in("kg", [128, 6])
    wsb_d = dram_in("w_sb_out", [SBW, D])
    wdl_d = dram_in("w_dil_out", [256, D])
    wout_d = dram_in("w_out", [D, D])
    g2_d = dram_in("g2", [128, D])
    wpq_d = dram_in("w_peer_q", [D, 2048])
    skT_d = dram_in("skT", [128, 16, 128])
    uv_d = dram_in("uv", [16384, 2048])
    consts_d = dram_in("consts", [128, C_END])
    alibi_d = dram_in("alibi", [6, 128, 1024])
    out_d = nc.dram_tensor("out", [NSEQ, S_LEN, D], F32, kind="ExternalOutput").ap()
    dbg_d = {}
    for name, shape in debug.get("outs", {}).items():
        dbg_d[name] = nc.dram_tensor(name, shape, F32, kind="ExternalOutput").ap()

    win_v = win_d.rearrange("(c p) n -> p c n", p=128)

    with ExitStack() as es_g:
        S = Sched(nc, es_g)

        uid = [0]

        def sb(es, name, shape, dt):
            uid[0] += 1
            return es.enter_context(nc.sbuf_tensor(f"s{uid[0]}_{name}", shape, dt))

        def pbank(es, name):
            uid[0] += 1
            return es.enter_context(nc.psum_tensor(f"p{uid[0]}_{name}", [128, 512], F32))

        cbf = sb(es_g, "cbf", [128, C_END], BF16)
        identf = sb(es_g, "identf", [128, 128], F32)
        g1 = sb(es_g, "g1sb", [128, D], F32)
        B_c = Buf("consts")
        S.dma("gpsimd", lambda e: e.dma_start(out=cbf[:], in_=consts_d), writes=[B_c])
        S.dma("sync", lambda e: e.dma_start(out=identf[:], in_=consts_d[:, C_IDENT:C_IDENT + 128]), writes=[B_c], sembuf=Buf())
        S.dma("sync", lambda e: e.dma_start(out=g1[:], in_=g1_d), writes=[B_c], sembuf=Buf())
        S.emit()
        ident = cbf[:, C_IDENT:C_IDENT + 128]
        negU = cbf[:, C_NEGU:C_NEGU + 128]
        negOne = cbf[:, C_NEGONE:C_NEGONE + 128]

        uvb = nc.dram_tensor("uvb", [16384, 2048], BF16).ap()
        B_uvb = Buf("uvb")

        wgb = nc.dram_tensor("wgb", [8, 128, 2048], BF16).ap()
        B_wgb = Buf("wgb")
        wgb_done = [False]

        def record_wgb():
            if wgb_done[0]:
                return
            wgb_done[0] = True
            for fc in range(8):
                for half in range(2):
                    col0 = 3 * SBW + 3 * DLW + half * D + fc * 128
                    S.dma("gpsimd", lambda e, fc=fc, half=half, col0=col0: e.dma_start(out=wgb[fc].rearrange("p (c j) -> p c j", j=256)[:, :, half * 128:(half + 1) * 128], in_=win_v[:, :, col0:col0 + 128]),
                          writes=[B_wgb], sembuf=B_wgb)

        precast_next = [0]

        def record_precast(n=64):
            record_wgb()
            i0 = precast_next[0]
            i1 = min(64, i0 + n)
            precast_next[0] = i1
            for i in range(i0, i1):
                S.dma("gpsimd", lambda e, i=i: e.dma_start(out=uvb[i * 256:(i + 1) * 256, :], in_=uv_d[i * 256:(i + 1) * 256, :]), writes=[B_uvb], sembuf=B_uvb)

        for sq in range(nseq):
            with ExitStack() as es_s:
                xnT = sb(es_s, "xnT", [128, 8, S_LEN], BF16)
                osbT = sb(es_s, "osbT", [128, 4, S_LEN], BF16)
                B_xnT = [Buf(f"xnT{i}") for i in range(32)]
                B_osb = [[Buf() for _ in range(8)] for _ in range(4)]

                with ExitStack() as es:
                    xt_r = Ring([sb(es, f"xt{i}", [128, D], F32) for i in range(3)])
                    xn_r = Ring([sb(es, f"xn{i}", [128, D], BF16) for i in range(3)])
                    junk_r = Ring([sb(es, f"junk{i}", [128, D], BF16) for i in range(2)])
                    st_r = Ring([sb(es, f"st{i}", [128, 4], F32) for i in range(3)])
                    ps_r = Ring([pbank(es, f"pst{i}") for i in range(3)])
                    a1ctx = [dict() for _ in range(32)]

                    def a1A(tt):
                        xt, Bxt = xt_r.next()
                        xn, Bxn = xn_r.next()
                        jk, Bjk = junk_r.next()
                        st, Bst = st_r.next()
                        ps, Bps = ps_r.next()
                        S.dma("sync", lambda e, xt=xt, tt=tt: e.dma_start(out=xt[:], in_=x_d[sq, tt * 128:(tt + 1) * 128, :]), writes=[Bxt])
                        S.op("scalar", lambda e, jk=jk, xt=xt, st=st: e.activation(out=jk[:], in_=xt[:], func=AF.Square, accum_out=st[:, 0:1]), reads=[Bxt], writes=[Bjk, Bst])
                        S.op("scalar", lambda e, st=st: e.activation(out=st[:, 1:2], in_=st[:, 0:1], func=AF.Sqrt, bias=EPS, scale=1.0 / D), reads=[Bst], writes=[Bst])
                        S.op("vector", lambda e, st=st: e.reciprocal(out=st[:, 2:3], in_=st[:, 1:2]), reads=[Bst], writes=[Bst])
                        S.op("vector", lambda e, xn=xn, xt=xt, st=st: e.scalar_tensor_tensor(out=xn[:], in0=xt[:], scalar=st[:, 2:3], in1=g1[:], op0=ALU.mult, op1=ALU.mult), reads=[Bxt, Bst], writes=[Bxn])
                        psb = ps[:].bitcast(BF16)
                        for c in range(8):
                            S.op("tensor", lambda e, psb=psb, xn=xn, c=c: e.transpose(out=psb[:, c * 128:(c + 1) * 128], in_=xn[:, c * 128:(c + 1) * 128], identity=ident), reads=[Bxn], writes=[Bps])
                        a1ctx[tt].update(psb=psb, Bps=Bps)

                    def a1B(tt):
                        psb, Bps = a1ctx[tt]["psb"], a1ctx[tt]["Bps"]
                        S.op("vector", lambda e, psb=psb, tt=tt: e.tensor_copy(out=xnT[:, :, tt * 128:(tt + 1) * 128], in_=psb.rearrange("p (c t) -> p c t", c=8)), reads=[Bps], writes=[B_xnT[tt]])

                    for t in range(33):
                        if t < 32:
                            a1A(t)
                        if t >= 1:
                            a1B(t - 1)
                    S.emit()

                if "xnT" in dbg_d and sq == 0:
                    with ExitStack() as es:
                        tmp = sb(es, "dbgx", [128, 8, 512], F32)
                        Bt = Buf()
                        S.op("vector", lambda e: e.tensor_copy(out=tmp[:], in_=xnT[:, :, 0:512]), writes=[Bt])
                        S.dma("sync", lambda e: e.dma_start(out=dbg_d["xnT"], in_=tmp[:]), reads=[Bt])
                        S.emit()

                npairs = debug.get("npairs", 4)
                with ExitStack() as es:
                    w_r = Ring([sb(es, f"wsl{i}", [128, 8, 128], BF16) for i in range(4)])
                    qT_r = Ring([sb(es, f"qT{i}", [128, S_LEN], BF16) for i in range(2)])
                    kT_r = Ring([sb(es, f"kT{i}", [128, S_LEN], BF16) for i in range(2)])
                    v_r = Ring([sb(es, f"vsb{i}", [128, 32, 128], BF16) for i in range(2)])
                    sp_r = Ring([sb(es, f"sp{i}", [128, QB], BF16) for i in range(4)])
                    e_r = Ring([sb(es, f"ef{i}", [128, QB], F32) for i in range(3)])
                    a_r = Ring([sb(es, f"a{i}", [128, QB], BF16) for i in range(4)])
                    ss_r = Ring([sb(es, f"spsum{i}", [128, QB], BF16) for i in range(3)])
                    zA_r = Ring([pbank(es, f"zA{i}") for i in range(2)])
                    zB_r = Ring([pbank(es, f"zB{i}") for i in range(2)])
                    oT_r = Ring([pbank(es, f"oT{i}") for i in range(2)])
                    pj_r = Ring([pbank(es, f"pj{i}") for i in range(2)])
                    mbig = cbf[:, C_MBIG:C_MBIG + 896]

                    def make_proj(pr):
                        R = []

                        def OP(*a_, **k_):
                            R.append(lambda: S.op(*a_, **k_))

                        def DMA(*a_, **k_):
                            R.append(lambda: S.dma(*a_, **k_))

                        qT, BqT = qT_r.next()
                        kT, BkT = kT_r.next()
                        vv, Bv = v_r.next()
                        for which, dst, Bdst, col0, scale in (("q", qT, BqT, pr * 128, 0.125), ("k", kT, BkT, SBW + pr * 128, 1.0)):
                            wsl, Bw = w_r.next()
                            DMA("gpsimd", lambda e, wsl=wsl, col0=col0: e.dma_start(out=wsl[:], in_=win_v[:, :, col0:col0 + 128]), writes=[Bw])
                            for tc in range(8):
                                pj, Bpj = pj_r.next()
                                for c in range(8):
                                    OP("tensor", lambda e, pj=pj, wsl=wsl, c=c, tc=tc: e.matmul(pj[:], lhsT=wsl[:, c, :], rhs=xnT[:, c, tc * 512:(tc + 1) * 512], start=(c == 0), stop=(c == 7)),
                                       reads=[Bw] + B_xnT[tc * 4:tc * 4 + 4], writes=[Bpj])
                                OP("vector", lambda e, pj=pj, dst=dst, tc=tc, scale=scale: e.tensor_scalar(out=dst[:, tc * 512:(tc + 1) * 512], in0=pj[:], scalar1=scale, scalar2=None, op0=ALU.mult), reads=[Bpj], writes=[Bdst])
                        wsl, Bw = w_r.next()
                        DMA("gpsimd", lambda e, wsl=wsl, pr=pr: e.dma_start(out=wsl[:], in_=win_v[:, :, 2 * SBW + pr * 128:2 * SBW + pr * 128 + 128]), writes=[Bw])
                        for tb4 in range(8):
                            pj, Bpj = pj_r.next()
                            for j in range(4):
                                tb = tb4 * 4 + j
                                for c in range(8):
                                    OP("tensor", lambda e, pj=pj, wsl=wsl, c=c, tb=tb, j=j: e.matmul(pj[:, j * 128:(j + 1) * 128], lhsT=xnT[:, c, tb * 128:(tb + 1) * 128], rhs=wsl[:, c, :], start=(c == 0), stop=(c == 7)),
                                       reads=[Bw, B_xnT[tb]], writes=[Bpj])
                            OP("vector", lambda e, pj=pj, vv=vv, tb4=tb4: e.tensor_copy(out=vv[:, tb4 * 4:tb4 * 4 + 4, :], in_=pj[:].rearrange("p (j d) -> p j d", j=4)), reads=[Bpj], writes=[Bv])
                        return (qT, BqT, kT, BkT, vv, Bv), R

                    cur_proj, R0 = make_proj(0) if npairs > 0 else (None, [])
                    for th in R0:
                        th()
                    for pr in range(npairs):
                        nxt_proj, nR = make_proj(pr + 1) if pr + 1 < npairs else (None, [])
                        nrk = 0
                        qT, BqT, kT, BkT, vv, Bv = cur_proj

                        if debug.get("peer", True):
                            record_precast(16)
                        tiles = []
                        for hh in range(2):
                            for qb in range(8):
                                kbs = list(range(4 * qb + 3, -1, -1))
                                for n, kb in enumerate(kbs):
                                    d = kb - 4 * qb
                                    c0 = 128 * d if d > 0 else 0
                                    tiles.append(dict(hh=hh, pb=hh * 64, qb=qb, q0=qb * QB, n=n, kb=kb, d=d, c0=c0, W=QB - c0, last=(n == len(kbs) - 1)))
                        chain = {}

                        def st1(T):
                            zA, BzA = zA_r.next()
                            T["zA"], T["BzA"] = zA, BzA
                            pb, kb, q0, c0, W = T["pb"], T["kb"], T["q0"], T["c0"], T["W"]
                            T["kTs"] = kT[pb:pb + 64, kb * 128:(kb + 1) * 128]
                            T["qTs"] = qT[pb:pb + 64, q0 + c0:q0 + QB]
                            S.op("tensor", lambda e, zA=zA, kTs=T["kTs"], qTs=T["qTs"], W=W: e.matmul(zA[:, 0:W], lhsT=kTs, rhs=qTs, start=True, stop=True), reads=[BkT, BqT], writes=[BzA])

                        def st2(T):
                            ef, Be = e_r.next()
                            sp, Bsp = sp_r.next()
                            T["sp"], T["Bsp"] = sp, Bsp
                            zA, BzA, W, d, c0 = T["zA"], T["BzA"], T["W"], T["d"], T["c0"]
                            S.op("scalar", lambda e, ef=ef, zA=zA, W=W: e.activation(out=ef[:, 0:W], in_=zA[:, 0:W], func=AF.Exp), reads=[BzA], writes=[Be])
                            S.op("scalar", lambda e, sp=sp, ef=ef, W=W: e.activation(out=sp[:, 0:W], in_=ef[:, 0:W], func=AF.Ln, bias=1.0), reads=[Be], writes=[Bsp])
                            if d >= 0:
                                msk = mbig[:, 384 - 128 * d + c0:384 - 128 * d + QB]
                                T["msk"] = msk
                                S.op("vector", lambda e, sp=sp, msk=msk, W=W: e.tensor_tensor(out=sp[:, 0:W], in0=sp[:, 0:W], in1=msk, op=ALU.mult), reads=[Bsp], writes=[Bsp])

                        def st3(T):
                            zB, BzB = zB_r.next()
                            T["zB"], T["BzB"] = zB, BzB
                            sp, Bsp, W, c0 = T["sp"], T["Bsp"], T["W"], T["c0"]
                            ch = chain.setdefault((T["hh"], T["qb"]), {"prev_ss": None})
                            prev_ss = ch["prev_ss"]
                            last_is_u = prev_ss is None
                            S.op("tensor", lambda e, zB=zB, kTs=T["kTs"], qTs=T["qTs"], W=W: e.matmul(zB[:, 0:W], lhsT=kTs, rhs=qTs, start=True, stop=False), reads=[BkT, BqT], writes=[BzB])
                            S.op("tensor", lambda e, zB=zB, sp=sp, W=W, last_is_u=last_is_u: e.matmul(zB[:, 0:W], lhsT=negU, rhs=sp[:, 0:W], start=False, stop=last_is_u), reads=[Bsp], writes=[BzB])
                            if prev_ss is not None:
                                pss, Bpss = prev_ss
                                S.op("tensor", lambda e, zB=zB, pss=pss, W=W, c0=c0: e.matmul(zB[:, 0:W], lhsT=negOne, rhs=pss[:, c0:QB], start=False, stop=True), reads=[Bpss], writes=[BzB])
                            if not T["last"]:
                                nss, Bnss = ss_r.next()
                                if prev_ss is None:
                                    if c0 > 0:
                                        S.op("gpsimd", lambda e, nss=nss, c0=c0: e.memset(nss[:, 0:c0], 0.0), writes=[Bnss])
                                    S.op("gpsimd", lambda e, nss=nss, sp=sp, c0=c0, W=W: e.tensor_copy(out=nss[:, c0:QB], in_=sp[:, 0:W]), reads=[Bsp], writes=[Bnss])
                                else:
                                    pss, Bpss = prev_ss
                                    if c0 > 0:
                                        S.op("gpsimd", lambda e, nss=nss, pss=pss, c0=c0: e.tensor_copy(out=nss[:, 0:c0], in_=pss[:, 0:c0]), reads=[Bpss], writes=[Bnss])
                                    S.op("vector", lambda e, nss=nss, pss=pss, sp=sp, c0=c0, W=W: e.tensor_tensor(out=nss[:, c0:QB], in0=pss[:, c0:QB], in1=sp[:, 0:W], op=ALU.add), reads=[Bpss, Bsp], writes=[Bnss])
                                ch["prev_ss"] = (nss, Bnss)

                        def st4(T):
                            aa, Ba = a_r.next()
                            T["aa"], T["Ba"] = aa, Ba
                            zB, BzB, W = T["zB"], T["BzB"], T["W"]
                            S.op("scalar", lambda e, aa=aa, zB=zB, W=W: e.activation(out=aa[:, 0:W], in_=zB[:, 0:W], func=AF.Exp), reads=[BzB], writes=[Ba])
                            if T["d"] >= 0:
                                S.op("vector", lambda e, aa=aa, msk=T["msk"], W=W: e.tensor_tensor(out=aa[:, 0:W], in0=aa[:, 0:W], in1=msk, op=ALU.mult), reads=[Ba], writes=[Ba])

                        def st5(T):
                            ch = chain[(T["hh"], T["qb"])]
                            if T["n"] == 0:
                                ch["oT"] = oT_r.next()
                            oT, BoT = ch["oT"]
                            aa, Ba, pb, c0, W, kb = T["aa"], T["Ba"], T["pb"], T["c0"], T["W"], T["kb"]
                            S.op("tensor", lambda e, oT=oT, aa=aa, kb=kb, pb=pb, c0=c0, W=W, first=(T["n"] == 0), last=T["last"], vv=vv: e.matmul(oT[pb:pb + 64, c0:QB], lhsT=vv[:, kb, pb:pb + 64], rhs=aa[:, 0:W], start=first, stop=last, skip_group_check=True),
                                 reads=[Bv, Ba], writes=[BoT])
                            if T["last"]:
                                S.op("vector", lambda e, oT=oT, pb=pb, q0=T["q0"], pr=pr: e.tensor_copy(out=osbT[pb:pb + 64, pr, q0:q0 + QB], in_=oT[pb:pb + 64, :]), reads=[BoT], writes=[B_osb[pr][T["qb"]]])

                        stages = (st1, st2, st3, st4, st5)
                        nsteps = len(tiles) + len(stages) - 1
                        for t in range(nsteps):
                            for si, fn in enumerate(stages):
                                i = t - si
                                if 0 <= i < len(tiles):
                                    fn(tiles[i])
                            tgt = (len(nR) * (t + 1)) // max(1, nsteps - 8)
                            while nrk < min(tgt, len(nR)):
                                nR[nrk]()
                                nrk += 1
                        while nrk < len(nR):
                            nR[nrk]()
                            nrk += 1
                        cur_proj = nxt_proj
                    S.emit()

                if "osbT" in dbg_d and sq == 0:
                    with ExitStack() as es:
                        tmp = sb(es, "dbgo", [128, S_LEN], F32)
                        Bt = Buf()
                        S.op("vector", lambda e: e.tensor_copy(out=tmp[:], in_=osbT[:, 0, :]), writes=[Bt])
                        S.dma("sync", lambda e: e.dma_start(out=dbg_d["osbT"], in_=tmp[:]), reads=[Bt])
                        S.emit()

                odlT = sb(es_s, "odlT", [128, 2, S_LEN], BF16)
                B_odl = [Buf() for _ in range(4)]
                ndil = debug.get("ndil", 4)
                with ExitStack() as es:
                    wq_r = Ring([sb(es, f"dwq{i}", [128, 8, 128], BF16) for i in range(3)])
                    dq = sb(es, "dq", [128, S_LEN], BF16)
                    dk = sb(es, "dk", [128, S_LEN], BF16)
                    Bdq, Bdk = Buf(), Buf()
                    vp = sb(es, "vperm", [128, 32, 128], BF16)
                    Bvp = Buf()
                    numacc = sb(es, "numacc", [128, S_LEN], F32)
                    denacc = sb(es, "denacc", [128, S_LEN], F32)
                    Bacc = Buf()
                    bt_r = Ring([sb(es, f"btile{i}", [128, 1024], F32) for i in range(1)])
                    sq_r = Ring([sb(es, f"dsq{i}", [128, 512], BF16) for i in range(2)])
                    ln_r = Ring([sb(es, f"dln{i}", [128, 512], F32) for i in range(2)])
                    ex_r = Ring([sb(es, f"dex{i}", [128, 512], F32) for i in range(3)])
                    pp_r = Ring([sb(es, f"dp{i}", [128, 512], BF16) for i in range(4)])
                    gq = sb(es, "gq8", [128, 6], F32)
                    gk = sb(es, "gk", [128, 6], F32)
                    bd = sb(es, "bdones", [128, 128], BF16)
                    Bg = Buf()
                    pj_r = Ring([pbank(es, f"dpj{i}") for i in range(3)])
                    ssum_ps = pbank(es, "dss")
                    Bssum = Buf()
                    sc_sets = [((pj_r.tiles[0], pj_r.bufs[0]), (pj_r.tiles[1], pj_r.bufs[1])),
                               ((pj_r.tiles[2], pj_r.bufs[2]), (ssum_ps, Bssum))]
                    sc_cnt = [0]
                    num_r = Ring([pbank(es, f"dnum{i}") for i in range(2)])
                    den_r = Ring([pbank(es, f"dden{i}") for i in range(2)])
                    ones_bf = cbf[:, C_ONES:C_ONES + 128]
                    S.dma("sync", lambda e: e.dma_start(out=gq[:], in_=qg_d), writes=[Bg])
                    S.dma("sync", lambda e: e.dma_start(out=gk[:], in_=kg_d), writes=[Bg], sembuf=Buf())
                    S.dma("gpsimd", lambda e: e.dma_start(out=bd[:], in_=consts_d[:, C_BD:C_BD + 128]), writes=[Bg], sembuf=Buf())
                    S.op("vector", lambda e: e.tensor_scalar(out=gq[:], in0=gq[:], scalar1=0.125, scalar2=None, op0=ALU.mult), reads=[Bg], writes=[Bg])
                    for jp in range(ndil // 2):
                        for g in range(3):
                            hA = 4 * g + 2 * jp
                            pi = hA // 2
                            r = DIL[g]
                            L = S_LEN // r
                            nqt = L // 128
                            for which, dst, Bdst, col0, gain in (("q", dq, Bdq, 3 * SBW + hA * 64, gq), ("k", dk, Bdk, 3 * SBW + DLW + hA * 64, gk)):
                                wsl, Bw = wq_r.next()
                                S.dma("gpsimd", lambda e, wsl=wsl, col0=col0: e.dma_start(out=wsl[:], in_=win_v[:, :, col0:col0 + 128]), writes=[Bw])
                                chunks = [dict(tc=tc) for tc in range(8)]

                                def pA(C, wsl=wsl, Bw=Bw):
                                    tc = C["tc"]
                                    pj, Bpj = pj_r.next()
                                    sqt, Bsq = sq_r.next()
                                    C.update(pj=pj, Bpj=Bpj, sqt=sqt, Bsq=Bsq)
                                    for c in range(8):
                                        S.op("tensor", lambda e, pj=pj, wsl=wsl, c=c, tc=tc: e.matmul(pj[:], lhsT=wsl[:, c, :], rhs=xnT[:, c, tc * 512:(tc + 1) * 512], start=(c == 0), stop=(c == 7)),
                                             reads=[Bw] + B_xnT[tc * 4:tc * 4 + 4], writes=[Bpj])
                                    S.op("scalar", lambda e, sqt=sqt, pj=pj: e.activation(out=sqt[:], in_=pj[:], func=AF.Square), reads=[Bpj], writes=[Bsq])

                                def pB(C, dst=dst, Bdst=Bdst, gain=gain, pi=pi):
                                    tc, pj, Bpj, sqt, Bsq = C["tc"], C["pj"], C["Bpj"], C["sqt"], C["Bsq"]
                                    lnt, Bln = ln_r.next()
                                    S.op("tensor", lambda e, sqt=sqt: e.matmul(ssum_ps[:], lhsT=bd[:], rhs=sqt[:], start=True, stop=True), reads=[Bsq, Bg], writes=[Bssum])
                                    S.op("scalar", lambda e, lnt=lnt: e.activation(out=lnt[:], in_=ssum_ps[:], func=AF.Ln, bias=EPS, scale=1.0 / 64), reads=[Bssum], writes=[Bln])
                                    S.op("scalar", lambda e, lnt=lnt: e.activation(out=lnt[:], in_=lnt[:], func=AF.Exp, scale=-0.5), reads=[Bln], writes=[Bln])
                                    S.op("vector", lambda e, dst=dst, pj=pj, lnt=lnt, gain=gain, pi=pi, tc=tc: e.scalar_tensor_tensor(out=dst[:, tc * 512:(tc + 1) * 512], in0=pj[:], scalar=gain[:, pi:pi + 1], in1=lnt[:], op0=ALU.mult, op1=ALU.mult),
                                         reads=[Bpj, Bln, Bg], writes=[Bdst])

                                for t in range(9):
                                    if t < 8:
                                        pA(chunks[t])
                                    if t >= 1:
                                        pB(chunks[t - 1])
                            wsl, Bw = wq_r.next()
                            vc0 = 3 * SBW + 2 * DLW + hA * 64
                            S.dma("gpsimd", lambda e, wsl=wsl, vc0=vc0: e.dma_start(out=wsl[:], in_=win_v[:, :, vc0:vc0 + 128]), writes=[Bw])
                            for b4 in range(8):
                                pj, Bpj = pj_r.next()
                                for jj in range(4):
                                    bi = b4 * 4 + jj
                                    cls, kb = bi // nqt, bi % nqt
                                    t0 = cls + r * kb * 128
                                    for c in range(8):
                                        S.op("tensor", lambda e, pj=pj, wsl=wsl, c=c, t0=t0, r=r, jj=jj: e.matmul(pj[:, jj * 128:(jj + 1) * 128], lhsT=xnT[:, c, t0:t0 + r * 127 + 1:r], rhs=wsl[:, c, :], start=(c == 0), stop=(c == 7)),
                                             reads=[Bw] + B_xnT, writes=[Bpj])
                                S.op("vector", lambda e, pj=pj, b4=b4: e.tensor_copy(out=vp[:, b4 * 4:(b4 + 1) * 4, :], in_=pj[:].rearrange("p (j d) -> p j d", j=4)), reads=[Bpj], writes=[Bvp])
                            bt, Bbt = bt_r.next()
                            S.dma("sync", lambda e, bt=bt, pi=pi: e.dma_start(out=bt[:], in_=alibi_d[pi]), writes=[Bbt])
                            nb = min(4, nqt)
                            pairs = []
                            for cls in range(r):
                                for qt0 in range(0, nqt, nb):
                                    for qi in range(nb):
                                        pairs.append(dict(cls=cls, qt0=qt0, qi=qi, qt=qt0 + qi, lastq=(qi == nb - 1)))
                            batch = {}

                            def a1(P, r=r):
                                cls, qt = P["cls"], P["qt"]
                                tq0 = cls + r * qt * 128
                                kd0 = tq0
                                kp0 = cls + r * (qt - 1) * 128 if qt > 0 else tq0
                                sset = sc_sets[sc_cnt[0] % 2]
                                sc_cnt[0] += 1
                                P["sset"] = sset
                                for e_ in range(2):
                                    pq_ = 64 * e_
                                    scv, Bs = sset[e_]
                                    qsl = dq[pq_:pq_ + 64, tq0:tq0 + r * 127 + 1:r]
                                    S.op("tensor", lambda e, scv=scv, kp0=kp0, r=r, qsl=qsl, pq_=pq_: e.matmul(scv[:, 0:128], lhsT=dk[pq_:pq_ + 64, kp0:kp0 + r * 127 + 1:r], rhs=qsl, start=True, stop=True), reads=[Bdk, Bdq], writes=[Bs])
                                    S.op("tensor", lambda e, scv=scv, kd0=kd0, r=r, qsl=qsl, pq_=pq_: e.matmul(scv[:, 128:256], lhsT=dk[pq_:pq_ + 64, kd0:kd0 + r * 127 + 1:r], rhs=qsl, start=True, stop=True), reads=[Bdk, Bdq], writes=[Bs])

                            def a2(P):
                                ex, Bex = ex_r.next()
                                P.update(ex=ex, Bex=Bex)
                                for e_ in range(2):
                                    scv, Bs = P["sset"][e_]
                                    S.op("scalar", lambda e, ex=ex, scv=scv, e_=e_: e.activation(out=ex[:, e_ * 256:(e_ + 1) * 256], in_=scv[:, 0:256], func=AF.Exp), reads=[Bs], writes=[Bex])

                            def a3(P, bt=bt, Bbt=Bbt):
                                pp, Bpp = pp_r.next()
                                P.update(pp=pp, Bpp=Bpp)
                                bsl = bt[:, 0:512] if P["qt"] > 0 else bt[:, 512:1024]
                                S.op("vector", lambda e, pp=pp, ex=P["ex"], bsl=bsl: e.tensor_tensor(out=pp[:], in0=ex[:], in1=bsl, op=ALU.mult), reads=[P["Bex"], Bbt], writes=[Bpp])

                            def a4(P, r=r, nqt=nqt, nb=nb, g=g):
                                cls, qt, qt0, qi = P["cls"], P["qt"], P["qt0"], P["qi"]
                                if qi == 0:
                                    batch[(cls, qt0)] = (num_r.next(), den_r.next())
                                (nump, Bnum), (denp, Bden) = batch[(cls, qt0)]
                                pp, Bpp = P["pp"], P["Bpp"]
                                bi_d = cls * nqt + qt
                                bi_p = cls * nqt + (qt - 1 if qt > 0 else qt)
                                osl = slice(qi * 128, (qi + 1) * 128)
                                for e_ in range(2):
                                    pq_ = 64 * e_
                                    c_ = e_ * 256
                                    S.op("tensor", lambda e, nump=nump, pp=pp, bi_p=bi_p, osl=osl, pq_=pq_, c_=c_: e.matmul(nump[pq_:pq_ + 64, osl], lhsT=vp[:, bi_p, pq_:pq_ + 64], rhs=pp[:, c_:c_ + 128], start=True, stop=False, skip_group_check=True), reads=[Bvp, Bpp], writes=[Bnum])
                                    S.op("tensor", lambda e, nump=nump, pp=pp, bi_d=bi_d, osl=osl, pq_=pq_, c_=c_: e.matmul(nump[pq_:pq_ + 64, osl], lhsT=vp[:, bi_d, pq_:pq_ + 64], rhs=pp[:, c_ + 128:c_ + 256], start=False, stop=True, skip_group_check=True), reads=[Bvp, Bpp], writes=[Bnum])
                                    S.op("tensor", lambda e, denp=denp, pp=pp, osl=osl, pq_=pq_, c_=c_: e.matmul(denp[pq_:pq_ + 64, osl], lhsT=ones_bf[:, 0:64], rhs=pp[:, c_:c_ + 128], start=True, stop=False, skip_group_check=True), reads=[Bpp], writes=[Bden])
                                    S.op("tensor", lambda e, denp=denp, pp=pp, osl=osl, pq_=pq_, c_=c_: e.matmul(denp[pq_:pq_ + 64, osl], lhsT=ones_bf[:, 0:64], rhs=pp[:, c_ + 128:c_ + 256], start=False, stop=True, skip_group_check=True), reads=[Bpp], writes=[Bden])
                                if P["lastq"]:
                                    ta = cls + r * qt0 * 128
                                    nt = nb * 128
                                    asl = slice(ta, ta + r * (nt - 1) + 1, r)
                                    if g == 0:
                                        S.op("vector", lambda e, nump=nump, asl=asl, nt=nt: e.tensor_copy(out=numacc[:, asl], in_=nump[:, 0:nt]), reads=[Bnum], writes=[Bacc])
                                        S.op("vector", lambda e, denp=denp, asl=asl, nt=nt: e.tensor_copy(out=denacc[:, asl], in_=denp[:, 0:nt]), reads=[Bden], writes=[Bacc])
                                    else:
                                        S.op("vector", lambda e, nump=nump, asl=asl, nt=nt: e.tensor_tensor(out=numacc[:, asl], in0=nump[:, 0:nt], in1=numacc[:, asl], op=ALU.add), reads=[Bnum, Bacc], writes=[Bacc])
                                        S.op("vector", lambda e, denp=denp, asl=asl, nt=nt: e.tensor_tensor(out=denacc[:, asl], in0=denp[:, 0:nt], in1=denacc[:, asl], op=ALU.add), reads=[Bden, Bacc], writes=[Bacc])

                            astages = (a1, a2, a3, a4)
                            if debug.get("a3_noattn"):
                                pairs = []
                            for t in range(len(pairs) + len(astages) - 1):
                                for si, fn in enumerate(astages):
                                    i = t - si
                                    if 0 <= i < len(pairs):
                                        fn(pairs[i])
                        S.op("vector", lambda e: e.reciprocal(out=denacc[:], in_=denacc[:]), reads=[Bacc], writes=[Bacc])
                        S.op("vector", lambda e, jp=jp: e.tensor_tensor(out=odlT[:, jp, :], in0=numacc[:], in1=denacc[:], op=ALU.mult), reads=[Bacc], writes=[B_odl[2 * jp], B_odl[2 * jp + 1]])
                    S.emit()

                if "odlT" in dbg_d and sq == 0:
                    with ExitStack() as es:
                        tmp = sb(es, "dbgd", [128, 2, S_LEN], F32)
                        Bt = Buf()
                        S.op("vector", lambda e: e.tensor_copy(out=tmp[:], in_=odlT[:]), writes=[Bt])
                        S.dma("sync", lambda e: e.dma_start(out=dbg_d["odlT"], in_=tmp[:]), reads=[Bt])
                        S.emit()

                if debug.get("a4", True):
                  with ExitStack() as es:
                    record_wgb()
                    wsb = sb(es, "wsb", [128, 4, D], BF16)
                    wdl = sb(es, "wdl", [128, 2, D], BF16)
                    wout = sb(es, "wout", [128, 8, D], BF16)
                    bgs = sb(es, "bgs", [128, 16], F32)
                    Bw4 = Buf()
                    S.dma("gpsimd", lambda e: e.dma_start(out=wsb[:], in_=wsb_d.rearrange("(c p) n -> p c n", p=128)), writes=[Bw4])
                    S.dma("gpsimd", lambda e: e.dma_start(out=wdl[:], in_=wdl_d.rearrange("(c p) n -> p c n", p=128)), writes=[Bw4], sembuf=Buf())
                    S.dma("gpsimd", lambda e: e.dma_start(out=wout[:], in_=wout_d.rearrange("(c p) n -> p c n", p=128)), writes=[Bw4], sembuf=Buf())
                    S.dma("sync", lambda e: e.dma_start(out=bgs[:], in_=bg_d), writes=[Bw4], sembuf=Buf())
                    wg_r = Ring([sb(es, f"wg{i}", [128, 8, 256], BF16) for i in range(3)])
                    gs_r = Ring([sb(es, f"gs{i}", [128, 512], F32) for i in range(2)])
                    gd_r = Ring([sb(es, f"gd{i}", [128, 512], F32) for i in range(2)])
                    m1_r = Ring([sb(es, f"m1{i}", [128, 512], F32) for i in range(2)])
                    m2_r = Ring([sb(es, f"m2{i}", [128, 512], F32) for i in range(2)])
                    mix_r = Ring([sb(es, f"mix{i}", [128, 8, 512], BF16) for i in range(2)])
                    xt_r = Ring([sb(es, f"xt4{i}", [128, D], F32) for i in range(2)])
                    x1_r = Ring([sb(es, f"x1{i}", [128, D], F32) for i in range(2)])
                    pa_r = Ring([pbank(es, f"pa{i}") for i in range(6)])
                    px_r = Ring([pbank(es, f"px{i}") for i in range(2)])
                    for tc in range(8):
                        tsl = slice(tc * 512, (tc + 1) * 512)
                        mix, Bmix = mix_r.next()
                        for fc in range(8):
                            fsl = slice(fc * 128, (fc + 1) * 128)
                            wg, Bwg = wg_r.next()
                            S.dma("sync", lambda e, wg=wg, fc=fc: e.dma_start(out=wg[:], in_=wgb[fc].rearrange("p (c j) -> p c j", j=256)), reads=[B_wgb], writes=[Bwg])
                            pys, Bpys = pa_r.next()
                            pyd, Bpyd = pa_r.next()
                            pgs, Bpgs = pa_r.next()
                            pgd, Bpgd = pa_r.next()
                            for c in range(4):
                                S.op("tensor", lambda e, pys=pys, c=c, fsl=fsl, tsl=tsl: e.matmul(pys[:], lhsT=wsb[:, c, fsl], rhs=osbT[:, c, tsl], start=(c == 0), stop=(c == 3)), reads=[Bw4, B_osb[c][tc]], writes=[Bpys])
                            for c in range(2):
                                S.op("tensor", lambda e, pyd=pyd, c=c, fsl=fsl, tsl=tsl: e.matmul(pyd[:], lhsT=wdl[:, c, fsl], rhs=odlT[:, c, tsl], start=(c == 0), stop=(c == 1)), reads=[Bw4] + B_odl, writes=[Bpyd])
                            for c in range(8):
                                S.op("tensor", lambda e, pgs=pgs, wg=wg, c=c, tsl=tsl: e.matmul(pgs[:], lhsT=wg[:, c, 0:128], rhs=xnT[:, c, tsl], start=(c == 0), stop=(c == 7)), reads=[Bwg] + B_xnT[tc * 4:tc * 4 + 4], writes=[Bpgs])
                            for c in range(8):
                                S.op("tensor", lambda e, pgd=pgd, wg=wg, c=c, tsl=tsl: e.matmul(pgd[:], lhsT=wg[:, c, 128:256], rhs=xnT[:, c, tsl], start=(c == 0), stop=(c == 7)), reads=[Bwg] + B_xnT[tc * 4:tc * 4 + 4], writes=[Bpgd])
                            gs, Bgs = gs_r.next()
                            gd, Bgd = gd_r.next()
                            m1, Bm1 = m1_r.next()
                            m2, Bm2 = m2_r.next()
                            S.op("scalar", lambda e, gs=gs, pgs=pgs, fc=fc: e.activation(out=gs[:], in_=pgs[:], func=AF.Sigmoid, bias=bgs[:, fc:fc + 1]), reads=[Bpgs, Bw4], writes=[Bgs])
                            S.op("scalar", lambda e, gd=gd, pgd=pgd, fc=fc: e.activation(out=gd[:], in_=pgd[:], func=AF.Sigmoid, bias=bgs[:, 8 + fc:9 + fc]), reads=[Bpgd, Bw4], writes=[Bgd])
                            S.op("vector", lambda e, m1=m1, pys=pys, gs=gs: e.tensor_tensor(out=m1[:], in0=pys[:], in1=gs[:], op=ALU.mult), reads=[Bpys, Bgs], writes=[Bm1])
                            S.op("vector", lambda e, m2=m2, pyd=pyd, gd=gd: e.tensor_tensor(out=m2[:], in0=pyd[:], in1=gd[:], op=ALU.mult), reads=[Bpyd, Bgd], writes=[Bm2])
                            S.op("gpsimd", lambda e, mix=mix, m1=m1, m2=m2, fc=fc: e.tensor_tensor(out=mix[:, fc, :], in0=m1[:], in1=m2[:], op=ALU.add), reads=[Bm1, Bm2], writes=[Bmix])
                        for t4 in range(4):
                            tt = tc * 4 + t4
                            xt, Bxt = xt_r.next()
                            x1, Bx1 = x1_r.next()
                            S.dma("sync", lambda e, xt=xt, tt=tt: e.dma_start(out=xt[:], in_=x_d[sq, tt * 128:(tt + 1) * 128, :]), writes=[Bxt])
                            for hf in range(2):
                                px, Bpx = px_r.next()
                                for fc in range(8):
                                    S.op("tensor", lambda e, px=px, mix=mix, fc=fc, t4=t4, hf=hf: e.matmul(px[:], lhsT=mix[:, fc, t4 * 128:(t4 + 1) * 128], rhs=wout[:, fc, hf * 512:(hf + 1) * 512], start=(fc == 0), stop=(fc == 7)), reads=[Bmix, Bw4], writes=[Bpx])
                                S.op("vector", lambda e, x1=x1, px=px, xt=xt, hf=hf: e.tensor_tensor(out=x1[:, hf * 512:(hf + 1) * 512], in0=px[:], in1=xt[:, hf * 512:(hf + 1) * 512], op=ALU.add), reads=[Bpx, Bxt], writes=[Bx1])
                            S.dma("sync", lambda e, x1=x1, tt=tt: e.dma_start(out=out_d[sq, tt * 128:(tt + 1) * 128, :], in_=x1[:]), reads=[Bx1], sembuf=Bx1)
                    S.emit()

        if debug.get("x1_in"):
            x1in_d = dram_in("x1in", [NSEQ, S_LEN, D])
            Bx = Buf()
            for i in range(8):
                S.dma("sync", lambda e, i=i: e.dma_start(out=out_d[0, i * 512:(i + 1) * 512, :], in_=x1in_d[0, i * 512:(i + 1) * 512, :]), writes=[Bx], sembuf=Bx)
            S.emit()

        if debug.get("peer", True):
          with ExitStack() as es:
            record_precast()
            wpq = sb(es, "wpq", [128, 8, 2048], BF16)
            skT = sb(es, "skT", [128, 16, 128], BF16)
            g2 = sb(es, "g2sb", [128, D], F32)
            iota_f = sb(es, "iota_f", [128, 16], F32)
            mhalf = sb(es, "mhalf", [128, 1], F32)
            econst = sb(es, "econst", [128, 128], F32)
            Bk = Buf()
            S.op("vector", lambda e: e.memset(mhalf[:], -0.5), writes=[Bk])
            S.op("vector", lambda e: e.memset(econst[:], float(np.e)), writes=[Bk])
            Bwp = Buf()
            wpq_v = wpq_d.rearrange("(c p) n -> p c n", p=128)
            for i in range(4):
                S.dma("gpsimd", lambda e, i=i: e.dma_start(out=wpq[:, :, i * 512:(i + 1) * 512], in_=wpq_v[:, :, i * 512:(i + 1) * 512]), writes=[Bwp], sembuf=Bwp)
            S.dma("gpsimd", lambda e: e.dma_start(out=skT[:], in_=skT_d), writes=[Bwp], sembuf=Bwp)
            S.dma("sync", lambda e: e.dma_start(out=g2[:], in_=g2_d), writes=[Bwp], sembuf=Buf())
            S.dma("sync", lambda e: e.dma_start(out=iota_f[:], in_=consts_d[:, C_IOTA:C_IOTA + 16]), writes=[Bwp], sembuf=Buf())

            x1_r = Ring([sb(es, f"bx1{i}", [128, D], F32) for i in range(3)])
            hnf_r = Ring([None, None])
            hnb_r = Ring([sb(es, f"hnb{i}", [128, D], BF16) for i in range(2)])
            prod_r = Ring([sb(es, f"prod{i}", [128, D], BF16) for i in range(5)])
            junkA = sb(es, "junkA", [128, D], BF16)
            hnT = sb(es, "hnT", [128, 8, 128], BF16); BhnT = Buf()
            qpT = sb(es, "qpT", [128, 16, 128], BF16); BqpT = Buf()
            s_sb = sb(es, "s_sb", [128, 16, 128], F32); Bs = Buf()
            tmpS = sb(es, "tmpS", [128, 16, 128], F32); Btmp = Buf()
            cand = s_sb[:].rearrange("p a b -> p (a b)").rearrange("p (h c) -> p h c", c=256); Bcand = Bs
            ctmp = tmpS[:].rearrange("p a b -> p (a b)").rearrange("p (h c) -> p h c", c=256); Bctmp = Btmp
            vals = sb(es, "vals", [128, 16, 16], F32); Bvals = Buf()
            idxu = sb(es, "idxu", [128, 16, 16], U32); Bidx = Buf()
            idxf = sb(es, "idxf", [128, 16, 16], F32); Bidxf = Buf()
            best = sb(es, "best", [128, 8, 16], F32); Bbest = Buf()
            ciu = sb(es, "ciu", [128, 8, 16], U32); Bci = Buf()
            hiu = sb(es, "hiu", [128, 8, 16], U32)
            lou = sb(es, "lou", [128, 8, 16], U32)
            hif = sb(es, "hif", [128, 8, 16], F32)
            lof = sb(es, "lof", [128, 8, 16], F32); Bhl = Buf()
            i12 = sb(es, "i12", [128, 2, 128], F32); Bi12 = Buf()
            ef = sb(es, "ef", [128, 128], F32); Bef = Buf()
            eidx_r = Ring([sb(es, f"eidx{i}", [128, 128], U32) for i in range(2)])
            gate_r = Ring([sb(es, f"gate{i}", [128, 128], F32) for i in range(2)])
            gtmp = sb(es, "gtmp", [128, 8, 16], F32); Bgt = Buf()
            gsum = sb(es, "gsum", [128, 16], F32); Bgs = Buf()
            a_r = Ring([sb(es, f"acol{i}", [128, 128], F32) for i in range(2)])
            ga_r = Ring([sb(es, f"gacol{i}", [128, 128], F32) for i in range(2)])
            st_r = Ring([sb(es, f"bst{i}", [128, 4], F32) for i in range(2)])
            junk = sb(es, "bjunk", [128, D], BF16); Bjunk = Buf()
            gb_r = Ring([sb(es, f"gb{i}", [128, 2048], BF16) for i in range(debug.get("ngb", 20))])
            dg_r = Ring([sb(es, f"dg{i}", [128, 128], BF16) for i in range(8)])
            psT = pbank(es, "bpsT"); BpsT = Buf()
            pq_r = Ring([pbank(es, f"bpq{i}") for i in range(4)])
            acc0 = pbank(es, "bacc0"); acc1 = pbank(es, "bacc1"); Bacc = Buf()

            ntiles = debug.get("peer_tiles", nseq * 32)
            STT_EVERY = debug.get("stt_every", 1000)
            print("PEER sbuf bytes remaining", nc.sbuf_bytes_remaining)

            def cp(dst, src):
                if debug.get("copy_eng", "scalar") == "scalar":
                    return ("scalar", lambda e: e.activation(out=dst, in_=src, func=AF.Copy))
                return ("vector", lambda e: e.tensor_copy(out=dst, in_=src))

            def make_sel(ti):
                R = []

                def OP(*a, **k):
                    R.append((a[0], lambda: S.op(*a, **k)))

                def DMA(*a, **k):
                    R.append((a[0], lambda: S.dma(*a, **k)))

                sq, tt = ti // 32, ti % 32
                rows = slice(tt * 128, (tt + 1) * 128)
                x1t, Bx1 = x1_r.next()
                hnf, Bhnf = hnf_r.next()
                hnb, Bhnb = hnb_r.next()
                st, Bst = st_r.next()
                eidx, Beidx = eidx_r.next()
                gate, Bgate = gate_r.next()
                acol, _ = a_r.next()
                gacol, _ = ga_r.next()
                DMA("sync", lambda e, x1t=x1t, sq=sq, rows=rows: e.dma_start(out=x1t[:], in_=out_d[sq, rows, :]), writes=[Bx1])
                OP("scalar", lambda e, x1t=x1t, st=st: e.activation(out=junk[:], in_=x1t[:], func=AF.Square, accum_out=st[:, 0:1]), reads=[Bx1], writes=[Bjunk, Bst])
                OP("scalar", lambda e, st=st: e.activation(out=st[:, 1:2], in_=st[:, 0:1], func=AF.Sqrt, bias=EPS, scale=1.0 / D), reads=[Bst], writes=[Bst])
                OP("vector", lambda e, st=st: e.reciprocal(out=st[:, 2:3], in_=st[:, 1:2]), reads=[Bst], writes=[Bst])
                OP("vector", lambda e, hnb=hnb, x1t=x1t, st=st: e.scalar_tensor_tensor(out=hnb[:], in0=x1t[:], scalar=st[:, 2:3], in1=g2[:], op0=ALU.mult, op1=ALU.mult), reads=[Bx1, Bst, Bwp], writes=[Bhnb])
                psb = psT[:].bitcast(BF16)
                for c in range(8):
                    OP("tensor", lambda e, c=c, hnb=hnb: e.transpose(out=psb[:, c * 128:(c + 1) * 128], in_=hnb[:, c * 128:(c + 1) * 128], identity=ident), reads=[Bhnb], writes=[BpsT])
                OP("vector", lambda e: e.tensor_copy(out=hnT[:], in_=psb.rearrange("p (c t) -> p c t", c=8)), reads=[BpsT], writes=[BhnT])
                for q4 in range(4):
                    pq, Bpq = pq_r.next()
                    for j in range(4):
                        hp = q4 * 4 + j
                        for c in range(8):
                            OP("tensor", lambda e, pq=pq, j=j, hp=hp, c=c: e.matmul(pq[:, j * 128:(j + 1) * 128], lhsT=wpq[:, c, hp * 128:(hp + 1) * 128], rhs=hnT[:, c, :], start=(c == 0), stop=(c == 7)), reads=[Bwp, BhnT], writes=[Bpq])
                    OP(*cp(qpT[:, q4 * 4:(q4 + 1) * 4, :], pq[:].rearrange("p (j t) -> p j t", j=4)), reads=[Bpq], writes=[BqpT])
                for q4 in range(4):
                    pq, Bpq = pq_r.next()
                    for j in range(4):
                        hp = q4 * 4 + j
                        OP("tensor", lambda e, pq=pq, j=j, hp=hp: e.matmul(pq[:, j * 128:(j + 1) * 128], lhsT=qpT[:, hp, :], rhs=skT[:, hp, :], start=True, stop=True), reads=[Bwp, BqpT], writes=[Bpq])
                    OP(*cp(s_sb[:, q4 * 4:(q4 + 1) * 4, :], pq[:].rearrange("p (j t) -> p j t", j=4)), reads=[Bpq], writes=[Bs])
                mark = len(R)
                for hp in range(16):
                    OP("vector", lambda e, hp=hp: e.max(out=vals[:, hp, 0:8], in_=s_sb[:, hp, :]), reads=[Bs], writes=[Bvals])
                    OP("vector", lambda e, hp=hp: e.match_replace(out=tmpS[:, hp, :], in_to_replace=vals[:, hp, 0:8], in_values=s_sb[:, hp, :], imm_value=-1e30), reads=[Bs, Bvals], writes=[Btmp])
                    OP("vector", lambda e, hp=hp: e.max(out=vals[:, hp, 8:16], in_=tmpS[:, hp, :]), reads=[Btmp], writes=[Bvals])
                    OP("vector", lambda e, hp=hp: e.max_index(out=idxu[:, hp, 0:8], in_max=vals[:, hp, 0:8], in_values=s_sb[:, hp, :]), reads=[Bs, Bvals], writes=[Bidx])
                    OP("vector", lambda e, hp=hp: e.max_index(out=idxu[:, hp, 8:16], in_max=vals[:, hp, 8:16], in_values=tmpS[:, hp, :]), reads=[Btmp, Bvals], writes=[Bidx])
                vv4 = vals[:].rearrange("p (h two) k -> p h two k", two=2)
                cand4 = cand.rearrange("p h (i j) -> p h i j", j=16)
                OP("vector", lambda e: e.tensor_tensor(out=cand4, in0=vv4[:, :, 0, :].unsqueeze(3).broadcast_to([128, 8, 16, 16]), in1=vv4[:, :, 1, :].unsqueeze(2).broadcast_to([128, 8, 16, 16]), op=ALU.add), reads=[Bvals], writes=[Bcand])
                for h in range(8):
                    OP("vector", lambda e, h=h: e.max(out=best[:, h, 0:8], in_=cand[:, h, :]), reads=[Bcand], writes=[Bbest])
                    OP("vector", lambda e, h=h: e.match_replace(out=ctmp[:, h, :], in_to_replace=best[:, h, 0:8], in_values=cand[:, h, :], imm_value=-1e30), reads=[Bcand, Bbest], writes=[Bctmp])
                    OP("vector", lambda e, h=h: e.max(out=best[:, h, 8:16], in_=ctmp[:, h, :]), reads=[Bctmp], writes=[Bbest])
                    OP("vector", lambda e, h=h: e.max_index(out=ciu[:, h, 0:8], in_max=best[:, h, 0:8], in_values=cand[:, h, :]), reads=[Bcand, Bbest], writes=[Bci])
                    OP("vector", lambda e, h=h: e.max_index(out=ciu[:, h, 8:16], in_max=best[:, h, 8:16], in_values=ctmp[:, h, :]), reads=[Bctmp, Bbest], writes=[Bci])
                OP("vector", lambda e: e.tensor_scalar(out=hiu[:], in0=ciu[:], scalar1=4, scalar2=None, op0=ALU.logical_shift_right), reads=[Bci], writes=[Bhl])
                OP("vector", lambda e: e.tensor_scalar(out=lou[:], in0=ciu[:], scalar1=15, scalar2=None, op0=ALU.bitwise_and), reads=[Bci], writes=[Bhl])
                OP("vector", lambda e: e.tensor_copy(out=hif[:], in_=hiu[:]), reads=[Bhl], writes=[Bhl])
                OP("vector", lambda e: e.tensor_copy(out=lof[:], in_=lou[:]), reads=[Bhl], writes=[Bhl])
                OP("vector", lambda e: e.tensor_copy(out=idxf[:], in_=idxu[:]), reads=[Bidx], writes=[Bidxf])
                if4 = idxf[:].rearrange("p (h two) k -> p h two k", two=2)
                oh4 = ctmp.rearrange("p h (k i) -> p h k i", i=16)
                iota_b = iota_f[:].unsqueeze(1).unsqueeze(1).broadcast_to([128, 8, 16, 16])
                for which, srcf, half in (("hi", hif, 0), ("lo", lof, 1)):
                    OP("vector", lambda e, srcf=srcf: e.tensor_tensor(out=oh4, in0=srcf[:].unsqueeze(3).broadcast_to([128, 8, 16, 16]), in1=iota_b, op=ALU.is_equal), reads=[Bhl, Bwp, Bctmp, Bci], writes=[Bctmp])
                    OP("vector", lambda e, half=half: e.tensor_tensor(out=oh4, in0=oh4, in1=if4[:, :, half, :].unsqueeze(2).broadcast_to([128, 8, 16, 16]), op=ALU.mult), reads=[Bctmp, Bidxf], writes=[Bctmp])
                    OP("vector", lambda e, half=half: e.tensor_reduce(out=i12[:, half, :], in_=ctmp.rearrange("p h (k i) -> p (h k) i", i=16), axis=AX.X, op=ALU.add), reads=[Bctmp], writes=[Bi12])
                OP("vector", lambda e: e.scalar_tensor_tensor(out=ef[:], in0=i12[:, 0, :], scalar=128.0, in1=i12[:, 1, :], op0=ALU.mult, op1=ALU.add), reads=[Bi12], writes=[Bef])
                OP("vector", lambda e, eidx=eidx: e.tensor_copy(out=eidx[:], in_=ef[:]), reads=[Bef], writes=[Beidx])
                OP("vector", lambda e: e.tensor_tensor(out=gtmp[:], in0=best[:], in1=best[:, :, 0:1].broadcast_to([128, 8, 16]), op=ALU.subtract), reads=[Bbest], writes=[Bgt])
                OP("scalar", lambda e: e.activation(out=gtmp[:], in_=gtmp[:], func=AF.Exp), reads=[Bgt], writes=[Bgt])
                OP("vector", lambda e: e.tensor_reduce(out=gsum[:, 0:8], in_=gtmp[:], axis=AX.X, op=ALU.add), reads=[Bgt], writes=[Bgs])
                OP("vector", lambda e: e.reciprocal(out=gsum[:, 8:16], in_=gsum[:, 0:8]), reads=[Bgs], writes=[Bgs])
                OP("vector", lambda e, gate=gate: e.tensor_tensor(out=gate[:].rearrange("p (h k) -> p h k", k=16), in0=gtmp[:], in1=gsum[:, 8:16].unsqueeze(2).broadcast_to([128, 8, 16]), op=ALU.mult), reads=[Bgt, Bgs], writes=[Bgate])

                ctx = dict(sq=sq, rows=rows, x1t=x1t, Bx1=Bx1, hnf=hnf, Bhnf=Bhnf, hnb=hnb, Bhnb=Bhnb, eidx=eidx, Beidx=Beidx, gate=gate, Bgate=Bgate, acol=acol, gacol=gacol)
                ctx["mark"] = mark
                return ctx, R

            ctx, R = make_sel(0)
            for _, th in R:
                th()
            for ti in range(ntiles):
                nctx, nR = make_sel(ti + 1) if ti + 1 < ntiles else (None, [])
                nk = 0
                sq, rows, x1t, Bx1, hnf, Bhnf = ctx["sq"], ctx["rows"], ctx["x1t"], ctx["Bx1"], ctx["hnf"], ctx["Bhnf"]
                hnb, Bhnb = ctx["hnb"], ctx["Bhnb"]
                eidx, Beidx, gate, Bgate, acol, gacol = ctx["eidx"], ctx["Beidx"], ctx["gate"], ctx["Bgate"], ctx["acol"], ctx["gacol"]
                Ba = [Buf() for _ in range(128)]
                Bga = [Buf() for _ in range(128)]
                items = [dict(hk=hk) for hk in range(128)]

                def itA(I):
                    hk = I["hk"]
                    gb, Bgb = gb_r.next()
                    I["gb"], I["Bgb"] = gb, Bgb
                    S.dma("gpsimd", lambda e, gb=gb, hk=hk, eidx=eidx: e.indirect_dma_start(out=gb[:], out_offset=None, in_=uvb, in_offset=bass.IndirectOffsetOnAxis(ap=eidx[:, hk:hk + 1], axis=0)), reads=[Beidx, B_uvb], writes=[Bgb])
                    I["fused"] = (hk % STT_EVERY == STT_EVERY - 1)
                    if I["fused"]:
                        S.op("vector", lambda e, gb=gb, hk=hk, hnb=hnb, acol=acol: e.scalar_tensor_tensor(out=junk[:], in0=gb[:, 0:D], scalar=1.0, in1=hnb[:], op0=ALU.mult, op1=ALU.mult, accum_out=acol[:, hk:hk + 1]), reads=[Bgb, Bhnb], writes=[Ba[hk]])
                        return
                    pd, Bpd = prod_r.next()
                    I["pd"], I["Bpd"] = pd, Bpd
                    S.op("vector", lambda e, gb=gb, pd=pd, hnb=hnb: e.tensor_tensor(out=pd[:], in0=gb[:, 0:D], in1=hnb[:], op=ALU.mult), reads=[Bgb, Bhnb], writes=[Bpd])

                def itA2(I):
                    hk = I["hk"]
                    if I["fused"]:
                        return
                    pd, Bpd = I["pd"], I["Bpd"]
                    S.op("scalar", lambda e, pd=pd, hk=hk, acol=acol: e.activation(out=junkA[:], in_=pd[:], func=AF.Copy, accum_out=acol[:, hk:hk + 1]), reads=[Bpd], writes=[Ba[hk]])

                def itB(I):
                    hk = I["hk"]
                    S.op("scalar", lambda e, hk=hk, acol=acol, gacol=gacol: e.activation(out=gacol[:, hk:hk + 1], in_=acol[:, hk:hk + 1], func=AF.Gelu), reads=[Ba[hk]], writes=[Bga[hk]])

                def itC(I):
                    hk = I["hk"]
                    gb, Bgb = I["gb"], I["Bgb"]
                    dg, Bdg = dg_r.next()
                    S.op(debug.get("dg_eng", "vector"), lambda e, dg=dg, hk=hk, gacol=gacol, gate=gate: e.tensor_scalar(out=dg[:], in0=ident, scalar1=gacol[:, hk:hk + 1], scalar2=gate[:, hk:hk + 1], op0=ALU.mult, op1=ALU.mult), reads=[Bga[hk], Bgate], writes=[Bdg])
                    S.op("tensor", lambda e, dg=dg, gb=gb, hk=hk: e.matmul(acc0[:], lhsT=dg[:], rhs=gb[:, D:D + 512], start=(hk == 0), stop=(hk == 127)), reads=[Bdg, Bgb], writes=[Bacc])
                    S.op("tensor", lambda e, dg=dg, gb=gb, hk=hk: e.matmul(acc1[:], lhsT=dg[:], rhs=gb[:, D + 512:D + 1024], start=(hk == 0), stop=(hk == 127)), reads=[Bdg, Bgb], writes=[Bacc])

                LAG_A2, LAG_B, LAG_C = debug.get("lags", (1, 2, 4))
                for t in range(128 + LAG_C):
                    if t < 128:
                        itA(items[t])
                    if 0 <= t - LAG_A2 < 128:
                        itA2(items[t - LAG_A2])
                    if 0 <= t - LAG_B < 128:
                        itB(items[t - LAG_B])
                    if 0 <= t - LAG_C < 128:
                        itC(items[t - LAG_C])
                    if nctx is not None:
                        nmark = nctx["mark"]
                        if nk < nmark:
                            ne, last_eng = 0, None
                            while nk < nmark and ne < 40:
                                eng_k, th = nR[nk]
                                if last_eng is not None and eng_k != last_eng:
                                    break
                                th()
                                last_eng = eng_k
                                nk += 1
                                ne += 1
                        else:
                            left_steps = max(1, 123 - t)
                            quota = -(-(len(nR) - nk) // left_steps)
                            for _ in range(quota):
                                if nk < len(nR):
                                    nR[nk][1]()
                                    nk += 1
                S.op("vector", lambda e, x1t=x1t: e.tensor_tensor(out=x1t[:, 0:512], in0=acc0[:], in1=x1t[:, 0:512], op=ALU.add), reads=[Bacc, Bx1], writes=[Bx1])
                S.op("vector", lambda e, x1t=x1t: e.tensor_tensor(out=x1t[:, 512:1024], in0=acc1[:], in1=x1t[:, 512:1024], op=ALU.add), reads=[Bacc, Bx1], writes=[Bx1])
                S.dma("sync", lambda e, x1t=x1t, sq=sq, rows=rows: e.dma_start(out=out_d[sq, rows, :], in_=x1t[:]), reads=[Bx1], sembuf=Bx1)
                ctx = nctx
            S.emit()
        print("total ops", S.total_ops, "sems", S.nsem)
    return nc


def prep_inputs(inputs):
    f = lambda a: np.ascontiguousarray(np.asarray(a, dtype=np.float32))
    x = f(inputs["x"])
    shared = {
        "g1": f(np.broadcast_to(np.asarray(inputs["norm1_gain"], np.float32)[0][None, :], (128, D))),
        "w_in": f(inputs["w_in"][0]),
        "b_gate": f(np.asarray(inputs["b_gate"], np.float32)[0].reshape(16, 128).T),
        "qg": f(np.asarray(inputs["q_norm_gain"], np.float32)[0].reshape(6, 128).T),
        "kg": f(np.asarray(inputs["k_norm_gain"], np.float32)[0].reshape(6, 128).T),
        "w_sb_out": f(inputs["w_sb_out"][0]),
        "w_dil_out": f(inputs["w_dil_out"][0]),
        "w_out": f(inputs["w_out"][0]),
        "g2": f(np.broadcast_to(np.asarray(inputs["norm2_gain"], np.float32)[0][None, :], (128, D))),
        "w_peer_q": f(inputs["w_peer_q"][0]),
        "skT": f(np.asarray(inputs["peer_sub_keys"], np.float32)[0].reshape(16, 128, 128).transpose(2, 0, 1)),
        "uv": f(np.concatenate([np.asarray(inputs["peer_u"], np.float32)[0], np.asarray(inputs["peer_v"], np.float32)[0]], axis=1)),
        "consts": host_consts(),
        "alibi": host_alibi(),
    }
    in_maps = []
    for c in range(NCORES):
        m = dict(shared)
        m["x"] = np.ascontiguousarray(x[c * NSEQ:(c + 1) * NSEQ])
        in_maps.append(m)
    return in_maps


def kernel(**inputs):
    in_maps = prep_inputs(inputs)
    nc = build()
    res = run_bass_kernel_spmd(nc, in_maps, core_ids=list(range(NCORES)))
    return np.concatenate([r["out"] for r in res.results], axis=0)
```

```python
from contextlib import ExitStack
import numpy as np
import concourse.bass as bass
import concourse.mybir as mybir
from concourse.bass_utils import run_bass_kernel_spmd

F32 = mybir.dt.float32
BF16 = mybir.dt.bfloat16
I32 = mybir.dt.int32
U32 = mybir.dt.uint32
AF = mybir.ActivationFunctionType
ALU = mybir.AluOpType
AX = mybir.AxisListType

ENGS = ("sync", "scalar", "vector", "gpsimd", "tensor")
SEM_LIMIT = 24000

S_LEN = 4096
D = 1024
NSEQ = 2
NCORES = 8
EPS = 1e-6
SBW = 512
DLW = 768
QB = 512
DIL = (1, 4, 16)


class Buf:
    __slots__ = ("name", "last_w", "readers", "dsem")

    def __init__(self, name=""):
        self.name = name
        self.last_w = None
        self.readers = []
        self.dsem = None


class Op:
    __slots__ = ("eng", "fn", "deps", "is_dma", "dsem", "sig", "need", "waits")

    def __init__(self, eng, fn, deps, is_dma, dsem):
        self.eng = eng
        self.fn = fn
        self.deps = deps
        self.is_dma = is_dma
        self.dsem = dsem
        self.sig = None
        self.need = False
        self.waits = None


class Sched:
    def __init__(self, nc, es):
        self.nc = nc
        self.es = es
        self.ops = []
        self.nsem = 0
        self.esem = {}
        self.ecnt = {}
        for e in ENGS:
            self.esem[e] = self._newsem("e_" + e)
            self.ecnt[e] = 0
        self.waited = {e: {} for e in ENGS}
        self.dcum = {}
        self.total_ops = 0

    def _newsem(self, name):
        self.nsem += 1
        return self.es.enter_context(self.nc.semaphore(f"{name}_{self.nsem}"))

    def dsem_for(self, buf):
        if buf.dsem is None:
            buf.dsem = self._newsem("d")
            self.dcum[buf.dsem] = 0
        return buf.dsem

    def share_dsem(self, bufs):
        s = self.dsem_for(bufs[0])
        for b in bufs[1:]:
            b.dsem = s

    def _deps(self, reads, writes):
        deps = []
        for b in reads:
            if b.last_w is not None:
                deps.append(b.last_w)
        for b in writes:
            if b.last_w is not None:
                deps.append(b.last_w)
            deps.extend(b.readers)
        return deps

    def _commit(self, op, reads, writes):
        for b in reads:
            b.readers.append(op)
        for b in writes:
            b.last_w = op
            b.readers = []
        self.ops.append(op)

    def op(self, eng, fn, reads=(), writes=()):
        o = Op(eng, fn, self._deps(reads, writes), False, None)
        self._commit(o, reads, writes)
        return o

    def dma(self, eng, fn, reads=(), writes=(), sembuf=None):
        if sembuf is None:
            sembuf = writes[0] if writes else reads[0]
        ds = self.dsem_for(sembuf)
        o = Op(eng, fn, self._deps(reads, writes), True, ds)
        self._commit(o, reads, writes)
        return o

    def emit(self, final_wait_eng="sync"):
        ops = self.ops
        for o in ops:
            for d in o.deps:
                if not (o.eng == "tensor" and d.eng == "tensor"):
                    d.need = True
        per_eng = {e: [] for e in ENGS}
        dma_seen = {}
        for o in ops:
            e = o.eng
            waits = []
            w = self.waited[e]
            for d in o.deps:
                if d.sig is None:
                    continue
                if e == "tensor" and d.eng == "tensor" and not d.is_dma:
                    continue
                s, v = d.sig
                if d.is_dma:
                    v = max(v, dma_seen.get(s, v))
                if w.get(s, 0) < v:
                    w[s] = v
                    waits.append((s, v))
            o.waits = waits
            if o.is_dma:
                self.dcum[o.dsem] += 16
                o.sig = (o.dsem, self.dcum[o.dsem])
                dma_seen[o.dsem] = self.dcum[o.dsem]
            elif o.need:
                if self.ecnt[e] >= SEM_LIMIT:
                    self.esem[e] = self._newsem("e_" + e)
                    self.ecnt[e] = 0
                self.ecnt[e] += 1
                o.sig = (self.esem[e], self.ecnt[e])
            per_eng[e].append(o)
        finals = list(dma_seen.items())
        nc = self.nc
        with nc.Block() as block:
            for e in ENGS:
                lst = per_eng[e]
                if not lst and e != final_wait_eng:
                    continue

                def body(eng, lst=lst, e=e):
                    for o in lst:
                        for (s, v) in o.waits:
                            eng.wait_ge(s, v)
                        ins = o.fn(eng)
                        if o.is_dma:
                            ins.then_inc(o.sig[0], 16)
                        elif o.sig is not None:
                            ins.then_inc(o.sig[0], 1)
                    if e == final_wait_eng:
                        for (s, v) in finals:
                            eng.wait_ge(s, v)

                getattr(block, e)(body)
        self.total_ops += len(ops)
        for o in ops:
            o.sig = None
        self.ops = []


class Ring:
    def __init__(self, tiles):
        self.tiles = tiles
        self.bufs = [Buf() for _ in tiles]
        self.i = 0

    def next(self):
        i = self.i % len(self.tiles)
        self.i += 1
        return self.tiles[i], self.bufs[i]


C_IDENT = 0
C_NEGU = 128
C_NEGONE = 256
C_MBIG = 384
C_ONES = 1280
C_IOTA = 1408
C_BD = 1424
C_END = 1552


def host_consts():
    c = np.zeros((128, C_END), np.float32)
    i = np.arange(128)[:, None]
    j = np.arange(128)[None, :]
    c[:, C_IDENT:C_IDENT + 128] = (i == j)
    c[:, C_NEGU:C_NEGU + 128] = -1.0 * (i >= j)
    c[:, C_NEGONE:C_NEGONE + 128] = -1.0
    cc = np.arange(896)[None, :]
    c[:, C_MBIG:C_MBIG + 896] = ((cc - 384) > i)
    c[:, C_ONES:C_ONES + 128] = 1.0
    c[:, C_IOTA:C_IOTA + 16] = np.arange(16)[None, :]
    c[:, C_BD:C_BD + 128] = ((i // 64) == (j // 64))
    return c


def host_alibi():
    slopes = 2.0 ** (-8.0 * np.arange(1, 13, dtype=np.float64) / 12.0)
    out = np.zeros((6, 128, 1024), np.float32)
    s = np.arange(128)[:, None].astype(np.float64)
    t = np.arange(128)[None, :].astype(np.float64)
    for h in range(12):
        r = DIL[h // 4]
        m = slopes[h] * r
        diag = np.where(t >= s, np.exp(-m * np.maximum(t - s, 0)), 0.0)
        prev = np.where(t <= s, np.exp(-m * (t - s + 128)), 0.0)
        pi, e = h // 2, h % 2
        out[pi, :, e * 256:e * 256 + 128] = prev
        out[pi, :, e * 256 + 128:e * 256 + 256] = diag
        out[pi, :, 512 + e * 256 + 128:512 + e * 256 + 256] = diag
    return out


def build(debug=None, nseq=NSEQ):
    debug = debug or {}
    nc = bass.Bass("TRN2", target_bir_lowering=False)
    dram_in = lambda name, shape, dt=F32: nc.dram_tensor(name, shape, dt, kind="ExternalInput").ap()
    x_d = dram_in("x", [NSEQ, S_LEN, D])
    g1_d = dram_in("g1", [128, D])
    win_d = dram_in("w_in", [D, 5888])
    bg_d = dram_in("b_gate", [128, 16])
    qg_d = dram_in("qg", [128, 6])
    kg_d = dram_in("kg", [128, 6])
    wsb_d = dram_in("w_sb_out", [SBW, D])
    wdl_d = dram_in("w_dil_out", [256, D])
    wout_d = dram_in("w_out", [D, D])
    g2_d = dram_in("g2", [128, D])
    wpq_d = dram_in("w_peer_q", [D, 2048])
    skT_d = dram_in("skT", [128, 16, 128])
    uv_d = dram_in("uv", [16384, 2048])
    consts_d = dram_in("consts", [128, C_END])
    alibi_d = dram_in("alibi", [6, 128, 1024])
    out_d = nc.dram_tensor("out", [NSEQ, S_LEN, D], F32, kind="ExternalOutput").ap()
    dbg_d = {}
    for name, shape in debug.get("outs", {}).items():
        dbg_d[name] = nc.dram_tensor(name, shape, F32, kind="ExternalOutput").ap()

    win_v = win_d.rearrange("(c p) n -> p c n", p=128)

    with ExitStack() as es_g:
        S = Sched(nc, es_g)

        uid = [0]

        def sb(es, name, shape, dt):
            uid[0] += 1
            return es.enter_context(nc.sbuf_tensor(f"s{uid[0]}_{name}", shape, dt))

        def pbank(es, name):
            uid[0] += 1
            return es.enter_context(nc.psum_tensor(f"p{uid[0]}_{name}", [128, 512], F32))

        cbf = sb(es_g, "cbf", [128, C_END], BF16)
        identf = sb(es_g, "identf", [128, 128], F32)
        g1 = sb(es_g, "g1sb", [128, D], F32)
        B_c = Buf("consts")
        S.dma("gpsimd", lambda e: e.dma_start(out=cbf[:], in_=consts_d), writes=[B_c])
        S.dma("sync", lambda e: e.dma_start(out=identf[:], in_=consts_d[:, C_IDENT:C_IDENT + 128]), writes=[B_c], sembuf=Buf())
        S.dma("sync", lambda e: e.dma_start(out=g1[:], in_=g1_d), writes=[B_c], sembuf=Buf())
        S.emit()
        ident = cbf[:, C_IDENT:C_IDENT + 128]
        negU = cbf[:, C_NEGU:C_NEGU + 128]
        negOne = cbf[:, C_NEGONE:C_NEGONE + 128]

        uvb = nc.dram_tensor("uvb", [16384, 2048], BF16).ap()
        B_uvb = Buf("uvb")

        wgb = nc.dram_tensor("wgb", [8, 128, 2048], BF16).ap()
        B_wgb = Buf("wgb")
        wgb_done = [False]

        def record_wgb():
            if wgb_done[0]:
                return
            wgb_done[0] = True
            for fc in range(8):
                for half in range(2):
                    col0 = 3 * SBW + 3 * DLW + half * D + fc * 128
                    S.dma("gpsimd", lambda e, fc=fc, half=half, col0=col0: e.dma_start(out=wgb[fc].rearrange("p (c j) -> p c j", j=256)[:, :, half * 128:(half + 1) * 128], in_=win_v[:, :, col0:col0 + 128]),
                          writes=[B_wgb], sembuf=B_wgb)

        precast_next = [0]

        def record_precast(n=64):
            record_wgb()
            i0 = precast_next[0]
            i1 = min(64, i0 + n)
            precast_next[0] = i1
            for i in range(i0, i1):
                S.dma("gpsimd", lambda e, i=i: e.dma_start(out=uvb[i * 256:(i + 1) * 256, :], in_=uv_d[i * 256:(i + 1) * 256, :]), writes=[B_uvb], sembuf=B_uvb)

        for sq in range(nseq):
            with ExitStack() as es_s:
                xnT = sb(es_s, "xnT", [128, 8, S_LEN], BF16)
                osbT = sb(es_s, "osbT", [128, 4, S_LEN], BF16)
                B_xnT = [Buf(f"xnT{i}") for i in range(32)]
                B_osb = [[Buf() for _ in range(8)] for _ in range(4)]

                with ExitStack() as es:
                    xt_r = Ring([sb(es, f"xt{i}", [128, D], F32) for i in range(3)])
                    xn_r = Ring([sb(es, f"xn{i}", [128, D], BF16) for i in range(2)])
                    junk_r = Ring([sb(es, f"junk{i}", [128, D], BF16) for i in range(2)])
                    st_r = Ring([sb(es, f"st{i}", [128, 4], F32) for i in range(3)])
                    ps_r = Ring([pbank(es, f"pst{i}") for i in range(2)])
                    for tt in range(32):
                        xt, Bxt = xt_r.next()
                        xn, Bxn = xn_r.next()
                        jk, Bjk = junk_r.next()
                        st, Bst = st_r.next()
                        ps, Bps = ps_r.next()
                        S.dma("sync", lambda e, xt=xt, tt=tt: e.dma_start(out=xt[:], in_=x_d[sq, tt * 128:(tt + 1) * 128, :]), writes=[Bxt])
                        S.op("scalar", lambda e, jk=jk, xt=xt, st=st: e.activation(out=jk[:], in_=xt[:], func=AF.Square, accum_out=st[:, 0:1]), reads=[Bxt], writes=[Bjk, Bst])
                        S.op("scalar", lambda e, st=st: e.activation(out=st[:, 1:2], in_=st[:, 0:1], func=AF.Sqrt, bias=EPS, scale=1.0 / D), reads=[Bst], writes=[Bst])
                        S.op("vector", lambda e, st=st: e.reciprocal(out=st[:, 2:3], in_=st[:, 1:2]), reads=[Bst], writes=[Bst])
                        S.op("vector", lambda e, xn=xn, xt=xt, st=st: e.scalar_tensor_tensor(out=xn[:], in0=xt[:], scalar=st[:, 2:3], in1=g1[:], op0=ALU.mult, op1=ALU.mult), reads=[Bxt, Bst], writes=[Bxn])
                        psb = ps[:].bitcast(BF16)
                        for c in range(8):
                            S.op("tensor", lambda e, psb=psb, xn=xn, c=c: e.transpose(out=psb[:, c * 128:(c + 1) * 128], in_=xn[:, c * 128:(c + 1) * 128], identity=ident), reads=[Bxn], writes=[Bps])
                        S.op("vector", lambda e, psb=psb, tt=tt: e.tensor_copy(out=xnT[:, :, tt * 128:(tt + 1) * 128], in_=psb.rearrange("p (c t) -> p c t", c=8)), reads=[Bps], writes=[B_xnT[tt]])
                    S.emit()

                if "xnT" in dbg_d and sq == 0:
                    with ExitStack() as es:
                        tmp = sb(es, "dbgx", [128, 8, 512], F32)
                        Bt = Buf()
                        S.op("vector", lambda e: e.tensor_copy(out=tmp[:], in_=xnT[:, :, 0:512]), writes=[Bt])
                        S.dma("sync", lambda e: e.dma_start(out=dbg_d["xnT"], in_=tmp[:]), reads=[Bt])
                        S.emit()

                npairs = debug.get("npairs", 4)
                with ExitStack() as es:
                    w_r = Ring([sb(es, f"wsl{i}", [128, 8, 128], BF16) for i in range(4)])
                    qT_r = Ring([sb(es, f"qT{i}", [128, S_LEN], BF16) for i in range(2)])
                    kT_r = Ring([sb(es, f"kT{i}", [128, S_LEN], BF16) for i in range(2)])
                    v_r = Ring([sb(es, f"vsb{i}", [128, 32, 128], BF16) for i in range(2)])
                    sp_r = Ring([sb(es, f"sp{i}", [128, QB], BF16) for i in range(4)])
                    e_r = Ring([sb(es, f"ef{i}", [128, QB], F32) for i in range(3)])
                    a_r = Ring([sb(es, f"a{i}", [128, QB], BF16) for i in range(4)])
                    ss_r = Ring([sb(es, f"spsum{i}", [128, QB], BF16) for i in range(3)])
                    zA_r = Ring([pbank(es, f"zA{i}") for i in range(2)])
                    zB_r = Ring([pbank(es, f"zB{i}") for i in range(2)])
                    oT_r = Ring([pbank(es, f"oT{i}") for i in range(2)])
                    pj_r = Ring([pbank(es, f"pj{i}") for i in range(2)])
                    mbig = cbf[:, C_MBIG:C_MBIG + 896]

                    def make_proj(pr):
                        R = []

                        def OP(*a_, **k_):
                            R.append(lambda: S.op(*a_, **k_))

                        def DMA(*a_, **k_):
                            R.append(lambda: S.dma(*a_, **k_))

                        qT, BqT = qT_r.next()
                        kT, BkT = kT_r.next()
                        vv, Bv = v_r.next()
                        for which, dst, Bdst, col0, scale in (("q", qT, BqT, pr * 128, 0.125), ("k", kT, BkT, SBW + pr * 128, 1.0)):
                            wsl, Bw = w_r.next()
                            DMA("gpsimd", lambda e, wsl=wsl, col0=col0: e.dma_start(out=wsl[:], in_=win_v[:, :, col0:col0 + 128]), writes=[Bw])
                            for tc in range(8):
                                pj, Bpj = pj_r.next()
                                for c in range(8):
                                    OP("tensor", lambda e, pj=pj, wsl=wsl, c=c, tc=tc: e.matmul(pj[:], lhsT=wsl[:, c, :], rhs=xnT[:, c, tc * 512:(tc + 1) * 512], start=(c == 0), stop=(c == 7)),
                                       reads=[Bw] + B_xnT[tc * 4:tc * 4 + 4], writes=[Bpj])
                                OP("vector", lambda e, pj=pj, dst=dst, tc=tc, scale=scale: e.tensor_scalar(out=dst[:, tc * 512:(tc + 1) * 512], in0=pj[:], scalar1=scale, scalar2=None, op0=ALU.mult), reads=[Bpj], writes=[Bdst])
                        wsl, Bw = w_r.next()
                        DMA("gpsimd", lambda e, wsl=wsl, pr=pr: e.dma_start(out=wsl[:], in_=win_v[:, :, 2 * SBW + pr * 128:2 * SBW + pr * 128 + 128]), writes=[Bw])
                        for tb4 in range(8):
                            pj, Bpj = pj_r.next()
                            for j in range(4):
                                tb = tb4 * 4 + j
                                for c in range(8):
                                    OP("tensor", lambda e, pj=pj, wsl=wsl, c=c, tb=tb, j=j: e.matmul(pj[:, j * 128:(j + 1) * 128], lhsT=xnT[:, c, tb * 128:(tb + 1) * 128], rhs=wsl[:, c, :], start=(c == 0), stop=(c == 7)),
                                       reads=[Bw, B_xnT[tb]], writes=[Bpj])
                            OP("vector", lambda e, pj=pj, vv=vv, tb4=tb4: e.tensor_copy(out=vv[:, tb4 * 4:tb4 * 4 + 4, :], in_=pj[:].rearrange("p (j d) -> p j d", j=4)), reads=[Bpj], writes=[Bv])
                        return (qT, BqT, kT, BkT, vv, Bv), R

                    cur_proj, R0 = make_proj(0) if npairs > 0 else (None, [])
                    for th in R0:
                        th()
                    for pr in range(npairs):
                        nxt_proj, nR = make_proj(pr + 1) if pr + 1 < npairs else (None, [])
                        nrk = 0
                        qT, BqT, kT, BkT, vv, Bv = cur_proj

                        if debug.get("peer", True):
                            record_precast(16)
                        tiles = []
                        for hh in range(2):
                            for qb in range(8):
                                kbs = list(range(4 * qb + 3, -1, -1))
                                for n, kb in enumerate(kbs):
                                    d = kb - 4 * qb
                                    c0 = 128 * d if d > 0 else 0
                                    tiles.append(dict(hh=hh, pb=hh * 64, qb=qb, q0=qb * QB, n=n, kb=kb, d=d, c0=c0, W=QB - c0, last=(n == len(kbs) - 1)))
                        chain = {}

                        def st1(T):
                            zA, BzA = zA_r.next()
                            T["zA"], T["BzA"] = zA, BzA
                            pb, kb, q0, c0, W = T["pb"], T["kb"], T["q0"], T["c0"], T["W"]
                            T["kTs"] = kT[pb:pb + 64, kb * 128:(kb + 1) * 128]
                            T["qTs"] = qT[pb:pb + 64, q0 + c0:q0 + QB]
                            S.op("tensor", lambda e, zA=zA, kTs=T["kTs"], qTs=T["qTs"], W=W: e.matmul(zA[:, 0:W], lhsT=kTs, rhs=qTs, start=True, stop=True), reads=[BkT, BqT], writes=[BzA])

                        def st2(T):
                            ef, Be = e_r.next()
                            sp, Bsp = sp_r.next()
                            T["sp"], T["Bsp"] = sp, Bsp
                            zA, BzA, W, d, c0 = T["zA"], T["BzA"], T["W"], T["d"], T["c0"]
                            S.op("scalar", lambda e, ef=ef, zA=zA, W=W: e.activation(out=ef[:, 0:W], in_=zA[:, 0:W], func=AF.Exp), reads=[BzA], writes=[Be])
                            S.op("scalar", lambda e, sp=sp, ef=ef, W=W: e.activation(out=sp[:, 0:W], in_=ef[:, 0:W], func=AF.Ln, bias=1.0), reads=[Be], writes=[Bsp])
                            if d >= 0:
                                msk = mbig[:, 384 - 128 * d + c0:384 - 128 * d + QB]
                                T["msk"] = msk
                                S.op("vector", lambda e, sp=sp, msk=msk, W=W: e.tensor_tensor(out=sp[:, 0:W], in0=sp[:, 0:W], in1=msk, op=ALU.mult), reads=[Bsp], writes=[Bsp])

                        def st3(T):
                            zB, BzB = zB_r.next()
                            T["zB"], T["BzB"] = zB, BzB
                            sp, Bsp, W, c0 = T["sp"], T["Bsp"], T["W"], T["c0"]
                            ch = chain.setdefault((T["hh"], T["qb"]), {"prev_ss": None})
                            prev_ss = ch["prev_ss"]
                            last_is_u = prev_ss is None
                            S.op("tensor", lambda e, zB=zB, kTs=T["kTs"], qTs=T["qTs"], W=W: e.matmul(zB[:, 0:W], lhsT=kTs, rhs=qTs, start=True, stop=False), reads=[BkT, BqT], writes=[BzB])
                            S.op("tensor", lambda e, zB=zB, sp=sp, W=W, last_is_u=last_is_u: e.matmul(zB[:, 0:W], lhsT=negU, rhs=sp[:, 0:W], start=False, stop=last_is_u), reads=[Bsp], writes=[BzB])
                            if prev_ss is not None:
                                pss, Bpss = prev_ss
                                S.op("tensor", lambda e, zB=zB, pss=pss, W=W, c0=c0: e.matmul(zB[:, 0:W], lhsT=negOne, rhs=pss[:, c0:QB], start=False, stop=True), reads=[Bpss], writes=[BzB])
                            if not T["last"]:
                                nss, Bnss = ss_r.next()
                                if prev_ss is None:
                                    if c0 > 0:
                                        S.op("gpsimd", lambda e, nss=nss, c0=c0: e.memset(nss[:, 0:c0], 0.0), writes=[Bnss])
                                    S.op("gpsimd", lambda e, nss=nss, sp=sp, c0=c0, W=W: e.tensor_copy(out=nss[:, c0:QB], in_=sp[:, 0:W]), reads=[Bsp], writes=[Bnss])
                                else:
                                    pss, Bpss = prev_ss
                                    if c0 > 0:
                                        S.op("gpsimd", lambda e, nss=nss, pss=pss, c0=c0: e.tensor_copy(out=nss[:, 0:c0], in_=pss[:, 0:c0]), reads=[Bpss], writes=[Bnss])
                                    S.op("vector", lambda e, nss=nss, pss=pss, sp=sp, c0=c0, W=W: e.tensor_tensor(out=nss[:, c0:QB], in0=pss[:, c0:QB], in1=sp[:, 0:W], op=ALU.add), reads=[Bpss, Bsp], writes=[Bnss])
                                ch["prev_ss"] = (nss, Bnss)

                        def st4(T):
                            aa, Ba = a_r.next()
                            T["aa"], T["Ba"] = aa, Ba
                            zB, BzB, W = T["zB"], T["BzB"], T["W"]
                            S.op("scalar", lambda e, aa=aa, zB=zB, W=W: e.activation(out=aa[:, 0:W], in_=zB[:, 0:W], func=AF.Exp), reads=[BzB], writes=[Ba])
                            if T["d"] >= 0:
                                S.op("vector", lambda e, aa=aa, msk=T["msk"], W=W: e.tensor_tensor(out=aa[:, 0:W], in0=aa[:, 0:W], in1=msk, op=ALU.mult), reads=[Ba], writes=[Ba])

                        def st5(T):
                            ch = chain[(T["hh"], T["qb"])]
                            if T["n"] == 0:
                                ch["oT"] = oT_r.next()
                            oT, BoT = ch["oT"]
                            aa, Ba, pb, c0, W, kb = T["aa"], T["Ba"], T["pb"], T["c0"], T["W"], T["kb"]
                            S.op("tensor", lambda e, oT=oT, aa=aa, kb=kb, pb=pb, c0=c0, W=W, first=(T["n"] == 0), last=T["last"], vv=vv: e.matmul(oT[pb:pb + 64, c0:QB], lhsT=vv[:, kb, pb:pb + 64], rhs=aa[:, 0:W], start=first, stop=last, skip_group_check=True),
                                 reads=[Bv, Ba], writes=[BoT])
                            if T["last"]:
                                S.op("vector", lambda e, oT=oT, pb=pb, q0=T["q0"], pr=pr: e.tensor_copy(out=osbT[pb:pb + 64, pr, q0:q0 + QB], in_=oT[pb:pb + 64, :]), reads=[BoT], writes=[B_osb[pr][T["qb"]]])

                        stages = (st1, st2, st3, st4, st5)
                        nsteps = len(tiles) + len(stages) - 1
                        for t in range(nsteps):
                            for si, fn in enumerate(stages):
                                i = t - si
                                if 0 <= i < len(tiles):
                                    fn(tiles[i])
                            tgt = (len(nR) * (t + 1)) // max(1, nsteps - 8)
                            while nrk < min(tgt, len(nR)):
                                nR[nrk]()
                                nrk += 1
                        while nrk < len(nR):
                            nR[nrk]()
                            nrk += 1
                        cur_proj = nxt_proj
                    S.emit()

                if "osbT" in dbg_d and sq == 0:
                    with ExitStack() as es:
                        tmp = sb(es, "dbgo", [128, S_LEN], F32)
                        Bt = Buf()
                        S.op("vector", lambda e: e.tensor_copy(out=tmp[:], in_=osbT[:, 0, :]), writes=[Bt])
                        S.dma("sync", lambda e: e.dma_start(out=dbg_d["osbT"], in_=tmp[:]), reads=[Bt])
                        S.emit()

                odlT = sb(es_s, "odlT", [128, 2, S_LEN], BF16)
                B_odl = [Buf() for _ in range(4)]
                ndil = debug.get("ndil", 4)
                with ExitStack() as es:
                    wq_r = Ring([sb(es, f"dwq{i}", [128, 8, 128], BF16) for i in range(3)])
                    dq = sb(es, "dq", [128, S_LEN], BF16)
                    dk = sb(es, "dk", [128, S_LEN], BF16)
                    Bdq, Bdk = Buf(), Buf()
                    vp = sb(es, "vperm", [128, 32, 128], BF16)
                    Bvp = Buf()
                    numacc = sb(es, "numacc", [128, S_LEN], F32)
                    denacc = sb(es, "denacc", [128, S_LEN], F32)
                    Bacc = Buf()
                    bt_r = Ring([sb(es, f"btile{i}", [128, 1024], F32) for i in range(1)])
                    sq_r = Ring([sb(es, f"dsq{i}", [128, 512], BF16) for i in range(2)])
                    ln_r = Ring([sb(es, f"dln{i}", [128, 512], F32) for i in range(2)])
                    ex_r = Ring([sb(es, f"dex{i}", [128, 512], F32) for i in range(3)])
                    pp_r = Ring([sb(es, f"dp{i}", [128, 512], BF16) for i in range(4)])
                    gq = sb(es, "gq8", [128, 6], F32)
                    gk = sb(es, "gk", [128, 6], F32)
                    bd = sb(es, "bdones", [128, 128], BF16)
                    Bg = Buf()
                    pj_r = Ring([pbank(es, f"dpj{i}") for i in range(3)])
                    ssum_ps = pbank(es, "dss")
                    Bssum = Buf()
                    sc_sets = [((pj_r.tiles[0], pj_r.bufs[0]), (pj_r.tiles[1], pj_r.bufs[1])),
                               ((pj_r.tiles[2], pj_r.bufs[2]), (ssum_ps, Bssum))]
                    sc_cnt = [0]
                    num_r = Ring([pbank(es, f"dnum{i}") for i in range(2)])
                    den_r = Ring([pbank(es, f"dden{i}") for i in range(2)])
                    ones_bf = cbf[:, C_ONES:C_ONES + 128]
                    S.dma("sync", lambda e: e.dma_start(out=gq[:], in_=qg_d), writes=[Bg])
                    S.dma("sync", lambda e: e.dma_start(out=gk[:], in_=kg_d), writes=[Bg], sembuf=Buf())
                    S.dma("gpsimd", lambda e: e.dma_start(out=bd[:], in_=consts_d[:, C_BD:C_BD + 128]), writes=[Bg], sembuf=Buf())
                    S.op("vector", lambda e: e.tensor_scalar(out=gq[:], in0=gq[:], scalar1=0.125, scalar2=None, op0=ALU.mult), reads=[Bg], writes=[Bg])
                    for jp in range(ndil // 2):
                        for g in range(3):
                            hA = 4 * g + 2 * jp
                            pi = hA // 2
                            r = DIL[g]
                            L = S_LEN // r
                            nqt = L // 128
                            for which, dst, Bdst, col0, gain in (("q", dq, Bdq, 3 * SBW + hA * 64, gq), ("k", dk, Bdk, 3 * SBW + DLW + hA * 64, gk)):
                                wsl, Bw = wq_r.next()
                                S.dma("gpsimd", lambda e, wsl=wsl, col0=col0: e.dma_start(out=wsl[:], in_=win_v[:, :, col0:col0 + 128]), writes=[Bw])
                                chunks = [dict(tc=tc) for tc in range(8)]

                                def pA(C, wsl=wsl, Bw=Bw):
                                    tc = C["tc"]
                                    pj, Bpj = pj_r.next()
                                    sqt, Bsq = sq_r.next()
                                    C.update(pj=pj, Bpj=Bpj, sqt=sqt, Bsq=Bsq)
                                    for c in range(8):
                                        S.op("tensor", lambda e, pj=pj, wsl=wsl, c=c, tc=tc: e.matmul(pj[:], lhsT=wsl[:, c, :], rhs=xnT[:, c, tc * 512:(tc + 1) * 512], start=(c == 0), stop=(c == 7)),
                                             reads=[Bw] + B_xnT[tc * 4:tc * 4 + 4], writes=[Bpj])
                                    S.op("scalar", lambda e, sqt=sqt, pj=pj: e.activation(out=sqt[:], in_=pj[:], func=AF.Square), reads=[Bpj], writes=[Bsq])

                                def pB(C, dst=dst, Bdst=Bdst, gain=gain, pi=pi):
                                    tc, pj, Bpj, sqt, Bsq = C["tc"], C["pj"], C["Bpj"], C["sqt"], C["Bsq"]
                                    lnt, Bln = ln_r.next()
                                    S.op("tensor", lambda e, sqt=sqt: e.matmul(ssum_ps[:], lhsT=bd[:], rhs=sqt[:], start=True, stop=True), reads=[Bsq, Bg], writes=[Bssum])
                                    S.op("scalar", lambda e, lnt=lnt: e.activation(out=lnt[:], in_=ssum_ps[:], func=AF.Ln, bias=EPS, scale=1.0 / 64), reads=[Bssum], writes=[Bln])
                                    S.op("scalar", lambda e, lnt=lnt: e.activation(out=lnt[:], in_=lnt[:], func=AF.Exp, scale=-0.5), reads=[Bln], writes=[Bln])
                                    S.op("vector", lambda e, dst=dst, pj=pj, lnt=lnt, gain=gain, pi=pi, tc=tc: e.scalar_tensor_tensor(out=dst[:, tc * 512:(tc + 1) * 512], in0=pj[:], scalar=gain[:, pi:pi + 1], in1=lnt[:], op0=ALU.mult, op1=ALU.mult),
                                         reads=[Bpj, Bln, Bg], writes=[Bdst])

                                for t in range(9):
                                    if t < 8:
                                        pA(chunks[t])
                                    if t >= 1:
                                        pB(chunks[t - 1])
                            wsl, Bw = wq_r.next()
                            vc0 = 3 * SBW + 2 * DLW + hA * 64
                            S.dma("gpsimd", lambda e, wsl=wsl, vc0=vc0: e.dma_start(out=wsl[:], in_=win_v[:, :, vc0:vc0 + 128]), writes=[Bw])
                            for b4 in range(8):
                                pj, Bpj = pj_r.next()
                                for jj in range(4):
                                    bi = b4 * 4 + jj
                                    cls, kb = bi // nqt, bi % nqt
                                    t0 = cls + r * kb * 128
                                    for c in range(8):
                                        S.op("tensor", lambda e, pj=pj, wsl=wsl, c=c, t0=t0, r=r, jj=jj: e.matmul(pj[:, jj * 128:(jj + 1) * 128], lhsT=xnT[:, c, t0:t0 + r * 127 + 1:r], rhs=wsl[:, c, :], start=(c == 0), stop=(c == 7)),
                                             reads=[Bw] + B_xnT, writes=[Bpj])
                                S.op("vector", lambda e, pj=pj, b4=b4: e.tensor_copy(out=vp[:, b4 * 4:(b4 + 1) * 4, :], in_=pj[:].rearrange("p (j d) -> p j d", j=4)), reads=[Bpj], writes=[Bvp])
                            bt, Bbt = bt_r.next()
                            S.dma("sync", lambda e, bt=bt, pi=pi: e.dma_start(out=bt[:], in_=alibi_d[pi]), writes=[Bbt])
                            nb = min(4, nqt)
                            pairs = []
                            for cls in range(r):
                                for qt0 in range(0, nqt, nb):
                                    for qi in range(nb):
                                        pairs.append(dict(cls=cls, qt0=qt0, qi=qi, qt=qt0 + qi, lastq=(qi == nb - 1)))
                            batch = {}

                            def a1(P, r=r):
                                cls, qt = P["cls"], P["qt"]
                                tq0 = cls + r * qt * 128
                                kd0 = tq0
                                kp0 = cls + r * (qt - 1) * 128 if qt > 0 else tq0
                                sset = sc_sets[sc_cnt[0] % 2]
                                sc_cnt[0] += 1
                                P["sset"] = sset
                                for e_ in range(2):
                                    pq_ = 64 * e_
                                    scv, Bs = sset[e_]
                                    qsl = dq[pq_:pq_ + 64, tq0:tq0 + r * 127 + 1:r]
                                    S.op("tensor", lambda e, scv=scv, kp0=kp0, r=r, qsl=qsl, pq_=pq_: e.matmul(scv[:, 0:128], lhsT=dk[pq_:pq_ + 64, kp0:kp0 + r * 127 + 1:r], rhs=qsl, start=True, stop=True), reads=[Bdk, Bdq], writes=[Bs])
                                    S.op("tensor", lambda e, scv=scv, kd0=kd0, r=r, qsl=qsl, pq_=pq_: e.matmul(scv[:, 128:256], lhsT=dk[pq_:pq_ + 64, kd0:kd0 + r * 127 + 1:r], rhs=qsl, start=True, stop=True), reads=[Bdk, Bdq], writes=[Bs])

                            def a2(P):
                                ex, Bex = ex_r.next()
                                P.update(ex=ex, Bex=Bex)
                                for e_ in range(2):
                                    scv, Bs = P["sset"][e_]
                                    S.op("scalar", lambda e, ex=ex, scv=scv, e_=e_: e.activation(out=ex[:, e_ * 256:(e_ + 1) * 256], in_=scv[:, 0:256], func=AF.Exp), reads=[Bs], writes=[Bex])

                            def a3(P, bt=bt, Bbt=Bbt):
                                pp, Bpp = pp_r.next()
                                P.update(pp=pp, Bpp=Bpp)
                                bsl = bt[:, 0:512] if P["qt"] > 0 else bt[:, 512:1024]
                                S.op("vector", lambda e, pp=pp, ex=P["ex"], bsl=bsl: e.tensor_tensor(out=pp[:], in0=ex[:], in1=bsl, op=ALU.mult), reads=[P["Bex"], Bbt], writes=[Bpp])

                            def a4(P, r=r, nqt=nqt, nb=nb, g=g):
                                cls, qt, qt0, qi = P["cls"], P["qt"], P["qt0"], P["qi"]
                                if qi == 0:
                                    batch[(cls, qt0)] = (num_r.next(), den_r.next())
                                (nump, Bnum), (denp, Bden) = batch[(cls, qt0)]
                                pp, Bpp = P["pp"], P["Bpp"]
                                bi_d = cls * nqt + qt
                                bi_p = cls * nqt + (qt - 1 if qt > 0 else qt)
                                osl = slice(qi * 128, (qi + 1) * 128)
                                for e_ in range(2):
                                    pq_ = 64 * e_
                                    c_ = e_ * 256
                                    S.op("tensor", lambda e, nump=nump, pp=pp, bi_p=bi_p, osl=osl, pq_=pq_, c_=c_: e.matmul(nump[pq_:pq_ + 64, osl], lhsT=vp[:, bi_p, pq_:pq_ + 64], rhs=pp[:, c_:c_ + 128], start=True, stop=False, skip_group_check=True), reads=[Bvp, Bpp], writes=[Bnum])
                                    S.op("tensor", lambda e, nump=nump, pp=pp, bi_d=bi_d, osl=osl, pq_=pq_, c_=c_: e.matmul(nump[pq_:pq_ + 64, osl], lhsT=vp[:, bi_d, pq_:pq_ + 64], rhs=pp[:, c_ + 128:c_ + 256], start=False, stop=True, skip_group_check=True), reads=[Bvp, Bpp], writes=[Bnum])
                                    S.op("tensor", lambda e, denp=denp, pp=pp, osl=osl, pq_=pq_, c_=c_: e.matmul(denp[pq_:pq_ + 64, osl], lhsT=ones_bf[:, 0:64], rhs=pp[:, c_:c_ + 128], start=True, stop=False, skip_group_check=True), reads=[Bpp], writes=[Bden])
                                    S.op("tensor", lambda e, denp=denp, pp=pp, osl=osl, pq_=pq_, c_=c_: e.matmul(denp[pq_:pq_ + 64, osl], lhsT=ones_bf[:, 0:64], rhs=pp[:, c_ + 128:c_ + 256], start=False, stop=True, skip_group_check=True), reads=[Bpp], writes=[Bden])
                                if P["lastq"]:
                                    ta = cls + r * qt0 * 128
                                    nt = nb * 128
                                    asl = slice(ta, ta + r * (nt - 1) + 1, r)
                                    if g == 0:
                                        S.op("vector", lambda e, nump=nump, asl=asl, nt=nt: e.tensor_copy(out=numacc[:, asl], in_=nump[:, 0:nt]), reads=[Bnum], writes=[Bacc])
                                        S.op("vector", lambda e, denp=denp, asl=asl, nt=nt: e.tensor_copy(out=denacc[:, asl], in_=denp[:, 0:nt]), reads=[Bden], writes=[Bacc])
                                    else:
                                        S.op("vector", lambda e, nump=nump, asl=asl, nt=nt: e.tensor_tensor(out=numacc[:, asl], in0=nump[:, 0:nt], in1=numacc[:, asl], op=ALU.add), reads=[Bnum, Bacc], writes=[Bacc])
                                        S.op("vector", lambda e, denp=denp, asl=asl, nt=nt: e.tensor_tensor(out=denacc[:, asl], in0=denp[:, 0:nt], in1=denacc[:, asl], op=ALU.add), reads=[Bden, Bacc], writes=[Bacc])

                            astages = (a1, a2, a3, a4)
                            if debug.get("a3_noattn"):
                                pairs = []
                            for t in range(len(pairs) + len(astages) - 1):
                                for si, fn in enumerate(astages):
                                    i = t - si
                                    if 0 <= i < len(pairs):
                                        fn(pairs[i])
                        S.op("vector", lambda e: e.reciprocal(out=denacc[:], in_=denacc[:]), reads=[Bacc], writes=[Bacc])
                        S.op("vector", lambda e, jp=jp: e.tensor_tensor(out=odlT[:, jp, :], in0=numacc[:], in1=denacc[:], op=ALU.mult), reads=[Bacc], writes=[B_odl[2 * jp], B_odl[2 * jp + 1]])
                    S.emit()

                if "odlT" in dbg_d and sq == 0:
                    with ExitStack() as es:
                        tmp = sb(es, "dbgd", [128, 2, S_LEN], F32)
                        Bt = Buf()
                        S.op("vector", lambda e: e.tensor_copy(out=tmp[:], in_=odlT[:]), writes=[Bt])
                        S.dma("sync", lambda e: e.dma_start(out=dbg_d["odlT"], in_=tmp[:]), reads=[Bt])
                        S.emit()

                if debug.get("a4", True):
                  with ExitStack() as es:
                    record_wgb()
                    wsb = sb(es, "wsb", [128, 4, D], BF16)
                    wdl = sb(es, "wdl", [128, 2, D], BF16)
                    wout = sb(es, "wout", [128, 8, D], BF16)
                    bgs = sb(es, "bgs", [128, 16], F32)
                    Bw4 = Buf()
                    S.dma("gpsimd", lambda e: e.dma_start(out=wsb[:], in_=wsb_d.rearrange("(c p) n -> p c n", p=128)), writes=[Bw4])
                    S.dma("gpsimd", lambda e: e.dma_start(out=wdl[:], in_=wdl_d.rearrange("(c p) n -> p c n", p=128)), writes=[Bw4], sembuf=Buf())
                    S.dma("gpsimd", lambda e: e.dma_start(out=wout[:], in_=wout_d.rearrange("(c p) n -> p c n", p=128)), writes=[Bw4], sembuf=Buf())
                    S.dma("sync", lambda e: e.dma_start(out=bgs[:], in_=bg_d), writes=[Bw4], sembuf=Buf())
                    wg_r = Ring([sb(es, f"wg{i}", [128, 8, 256], BF16) for i in range(3)])
                    gs_r = Ring([sb(es, f"gs{i}", [128, 512], F32) for i in range(2)])
                    gd_r = Ring([sb(es, f"gd{i}", [128, 512], F32) for i in range(2)])
                    m1_r = Ring([sb(es, f"m1{i}", [128, 512], F32) for i in range(2)])
                    m2_r = Ring([sb(es, f"m2{i}", [128, 512], F32) for i in range(2)])
                    mix_r = Ring([sb(es, f"mix{i}", [128, 8, 512], BF16) for i in range(2)])
                    xt_r = Ring([sb(es, f"xt4{i}", [128, D], F32) for i in range(2)])
                    x1_r = Ring([sb(es, f"x1{i}", [128, D], F32) for i in range(2)])
                    pa_r = Ring([pbank(es, f"pa{i}") for i in range(6)])
                    px_r = Ring([pbank(es, f"px{i}") for i in range(2)])
                    for tc in range(8):
                        tsl = slice(tc * 512, (tc + 1) * 512)
                        mix, Bmix = mix_r.next()
                        for fc in range(8):
                            fsl = slice(fc * 128, (fc + 1) * 128)
                            wg, Bwg = wg_r.next()
                            S.dma("sync", lambda e, wg=wg, fc=fc: e.dma_start(out=wg[:], in_=wgb[fc].rearrange("p (c j) -> p c j", j=256)), reads=[B_wgb], writes=[Bwg])
                            pys, Bpys = pa_r.next()
                            pyd, Bpyd = pa_r.next()
                            pgs, Bpgs = pa_r.next()
                            pgd, Bpgd = pa_r.next()
                            for c in range(4):
                                S.op("tensor", lambda e, pys=pys, c=c, fsl=fsl, tsl=tsl: e.matmul(pys[:], lhsT=wsb[:, c, fsl], rhs=osbT[:, c, tsl], start=(c == 0), stop=(c == 3)), reads=[Bw4, B_osb[c][tc]], writes=[Bpys])
                            for c in range(2):
                                S.op("tensor", lambda e, pyd=pyd, c=c, fsl=fsl, tsl=tsl: e.matmul(pyd[:], lhsT=wdl[:, c, fsl], rhs=odlT[:, c, tsl], start=(c == 0), stop=(c == 1)), reads=[Bw4] + B_odl, writes=[Bpyd])
                            for c in range(8):
                                S.op("tensor", lambda e, pgs=pgs, wg=wg, c=c, tsl=tsl: e.matmul(pgs[:], lhsT=wg[:, c, 0:128], rhs=xnT[:, c, tsl], start=(c == 0), stop=(c == 7)), reads=[Bwg] + B_xnT[tc * 4:tc * 4 + 4], writes=[Bpgs])
                            for c in range(8):
                                S.op("tensor", lambda e, pgd=pgd, wg=wg, c=c, tsl=tsl: e.matmul(pgd[:], lhsT=wg[:, c, 128:256], rhs=xnT[:, c, tsl], start=(c == 0), stop=(c == 7)), reads=[Bwg] + B_xnT[tc * 4:tc * 4 + 4], writes=[Bpgd])
                            gs, Bgs = gs_r.next()
                            gd, Bgd = gd_r.next()
                            m1, Bm1 = m1_r.next()
                            m2, Bm2 = m2_r.next()
                            S.op("scalar", lambda e, gs=gs, pgs=pgs, fc=fc: e.activation(out=gs[:], in_=pgs[:], func=AF.Sigmoid, bias=bgs[:, fc:fc + 1]), reads=[Bpgs, Bw4], writes=[Bgs])
                            S.op("scalar", lambda e, gd=gd, pgd=pgd, fc=fc: e.activation(out=gd[:], in_=pgd[:], func=AF.Sigmoid, bias=bgs[:, 8 + fc:9 + fc]), reads=[Bpgd, Bw4], writes=[Bgd])
                            S.op("vector", lambda e, m1=m1, pys=pys, gs=gs: e.tensor_tensor(out=m1[:], in0=pys[:], in1=gs[:], op=ALU.mult), reads=[Bpys, Bgs], writes=[Bm1])
                            S.op("vector", lambda e, m2=m2, pyd=pyd, gd=gd: e.tensor_tensor(out=m2[:], in0=pyd[:], in1=gd[:], op=ALU.mult), reads=[Bpyd, Bgd], writes=[Bm2])
                            S.op("gpsimd", lambda e, mix=mix, m1=m1, m2=m2, fc=fc: e.tensor_tensor(out=mix[:, fc, :], in0=m1[:], in1=m2[:], op=ALU.add), reads=[Bm1, Bm2], writes=[Bmix])
                        for t4 in range(4):
                            tt = tc * 4 + t4
                            xt, Bxt = xt_r.next()
                            x1, Bx1 = x1_r.next()
                            S.dma("sync", lambda e, xt=xt, tt=tt: e.dma_start(out=xt[:], in_=x_d[sq, tt * 128:(tt + 1) * 128, :]), writes=[Bxt])
                            for hf in range(2):
                                px, Bpx = px_r.next()
                                for fc in range(8):
                                    S.op("tensor", lambda e, px=px, mix=mix, fc=fc, t4=t4, hf=hf: e.matmul(px[:], lhsT=mix[:, fc, t4 * 128:(t4 + 1) * 128], rhs=wout[:, fc, hf * 512:(hf + 1) * 512], start=(fc == 0), stop=(fc == 7)), reads=[Bmix, Bw4], writes=[Bpx])
                                S.op("vector", lambda e, x1=x1, px=px, xt=xt, hf=hf: e.tensor_tensor(out=x1[:, hf * 512:(hf + 1) * 512], in0=px[:], in1=xt[:, hf * 512:(hf + 1) * 512], op=ALU.add), reads=[Bpx, Bxt], writes=[Bx1])
                            S.dma("sync", lambda e, x1=x1, tt=tt: e.dma_start(out=out_d[sq, tt * 128:(tt + 1) * 128, :], in_=x1[:]), reads=[Bx1], sembuf=Bx1)
                    S.emit()

        if debug.get("x1_in"):
            x1in_d = dram_in("x1in", [NSEQ, S_LEN, D])
            Bx = Buf()
            for i in range(8):
                S.dma("sync", lambda e, i=i: e.dma_start(out=out_d[0, i * 512:(i + 1) * 512, :], in_=x1in_d[0, i * 512:(i + 1) * 512, :]), writes=[Bx], sembuf=Bx)
            S.emit()

        if debug.get("peer", True):
          with ExitStack() as es:
            record_precast()
            wpq = sb(es, "wpq", [128, 8, 2048], BF16)
            skT = sb(es, "skT", [128, 16, 128], BF16)
            g2 = sb(es, "g2sb", [128, D], F32)
            iota_f = sb(es, "iota_f", [128, 16], F32)
            mhalf = sb(es, "mhalf", [128, 1], F32)
            econst = sb(es, "econst", [128, 128], F32)
            Bk = Buf()
            S.op("vector", lambda e: e.memset(mhalf[:], -0.5), writes=[Bk])
            S.op("vector", lambda e: e.memset(econst[:], float(np.e)), writes=[Bk])
            Bwp = Buf()
            wpq_v = wpq_d.rearrange("(c p) n -> p c n", p=128)
            for i in range(4):
                S.dma("gpsimd", lambda e, i=i: e.dma_start(out=wpq[:, :, i * 512:(i + 1) * 512], in_=wpq_v[:, :, i * 512:(i + 1) * 512]), writes=[Bwp], sembuf=Bwp)
            S.dma("gpsimd", lambda e: e.dma_start(out=skT[:], in_=skT_d), writes=[Bwp], sembuf=Bwp)
            S.dma("sync", lambda e: e.dma_start(out=g2[:], in_=g2_d), writes=[Bwp], sembuf=Buf())
            S.dma("sync", lambda e: e.dma_start(out=iota_f[:], in_=consts_d[:, C_IOTA:C_IOTA + 16]), writes=[Bwp], sembuf=Buf())

            x1_r = Ring([sb(es, f"bx1{i}", [128, D], F32) for i in range(3)])
            hnf_r = Ring([None, None])
            hnb_r = Ring([sb(es, f"hnb{i}", [128, D], BF16) for i in range(2)])
            prod_r = Ring([sb(es, f"prod{i}", [128, D], BF16) for i in range(5)])
            junkA = sb(es, "junkA", [128, D], BF16)
            hnT = sb(es, "hnT", [128, 8, 128], BF16); BhnT = Buf()
            qpT = sb(es, "qpT", [128, 16, 128], BF16); BqpT = Buf()
            s_sb = sb(es, "s_sb", [128, 16, 128], F32); Bs = Buf()
            tmpS = sb(es, "tmpS", [128, 16, 128], F32); Btmp = Buf()
            cand = s_sb[:].rearrange("p a b -> p (a b)").rearrange("p (h c) -> p h c", c=256); Bcand = Bs
            ctmp = tmpS[:].rearrange("p a b -> p (a b)").rearrange("p (h c) -> p h c", c=256); Bctmp = Btmp
            vals = sb(es, "vals", [128, 16, 16], F32); Bvals = Buf()
            idxu = sb(es, "idxu", [128, 16, 16], U32); Bidx = Buf()
            idxf = sb(es, "idxf", [128, 16, 16], F32); Bidxf = Buf()
            best = sb(es, "best", [128, 8, 16], F32); Bbest = Buf()
            ciu = sb(es, "ciu", [128, 8, 16], U32); Bci = Buf()
            hiu = sb(es, "hiu", [128, 8, 16], U32)
            lou = sb(es, "lou", [128, 8, 16], U32)
            hif = sb(es, "hif", [128, 8, 16], F32)
            lof = sb(es, "lof", [128, 8, 16], F32); Bhl = Buf()
            i12 = sb(es, "i12", [128, 2, 128], F32); Bi12 = Buf()
            ef = sb(es, "ef", [128, 128], F32); Bef = Buf()
            eidx_r = Ring([sb(es, f"eidx{i}", [128, 128], U32) for i in range(2)])
            gate_r = Ring([sb(es, f"gate{i}", [128, 128], F32) for i in range(2)])
            gtmp = sb(es, "gtmp", [128, 8, 16], F32); Bgt = Buf()
            gsum = sb(es, "gsum", [128, 16], F32); Bgs = Buf()
            a_r = Ring([sb(es, f"acol{i}", [128, 128], F32) for i in range(2)])
            ga_r = Ring([sb(es, f"gacol{i}", [128, 128], F32) for i in range(2)])
            st_r = Ring([sb(es, f"bst{i}", [128, 4], F32) for i in range(2)])
            junk = sb(es, "bjunk", [128, D], BF16); Bjunk = Buf()
            gb_r = Ring([sb(es, f"gb{i}", [128, 2048], BF16) for i in range(debug.get("ngb", 23))])
            dg_r = Ring([sb(es, f"dg{i}", [128, 128], BF16) for i in range(8)])
            psT = pbank(es, "bpsT"); BpsT = Buf()
            pq_r = Ring([pbank(es, f"bpq{i}") for i in range(4)])
            acc0 = pbank(es, "bacc0"); acc1 = pbank(es, "bacc1"); Bacc = Buf()

            ntiles = debug.get("peer_tiles", nseq * 32)
            STT_EVERY = debug.get("stt_every", 1000)
            print("PEER sbuf bytes remaining", nc.sbuf_bytes_remaining)

            def cp(dst, src):
                if debug.get("copy_eng", "scalar") == "scalar":
                    return ("scalar", lambda e: e.activation(out=dst, in_=src, func=AF.Copy))
                return ("vector", lambda e: e.tensor_copy(out=dst, in_=src))

            def make_sel(ti):
                R = []

                def OP(*a, **k):
                    R.append((a[0], lambda: S.op(*a, **k)))

                def DMA(*a, **k):
                    R.append((a[0], lambda: S.dma(*a, **k)))

                sq, tt = ti // 32, ti % 32
                rows = slice(tt * 128, (tt + 1) * 128)
                x1t, Bx1 = x1_r.next()
                hnf, Bhnf = hnf_r.next()
                hnb, Bhnb = hnb_r.next()
                st, Bst = st_r.next()
                eidx, Beidx = eidx_r.next()
                gate, Bgate = gate_r.next()
                acol, _ = a_r.next()
                gacol, _ = ga_r.next()
                DMA("sync", lambda e, x1t=x1t, sq=sq, rows=rows: e.dma_start(out=x1t[:], in_=out_d[sq, rows, :]), writes=[Bx1])
                OP("scalar", lambda e, x1t=x1t, st=st: e.activation(out=junk[:], in_=x1t[:], func=AF.Square, accum_out=st[:, 0:1]), reads=[Bx1], writes=[Bjunk, Bst])
                OP("scalar", lambda e, st=st: e.activation(out=st[:, 1:2], in_=st[:, 0:1], func=AF.Sqrt, bias=EPS, scale=1.0 / D), reads=[Bst], writes=[Bst])
                OP("vector", lambda e, st=st: e.reciprocal(out=st[:, 2:3], in_=st[:, 1:2]), reads=[Bst], writes=[Bst])
                OP("vector", lambda e, hnb=hnb, x1t=x1t, st=st: e.scalar_tensor_tensor(out=hnb[:], in0=x1t[:], scalar=st[:, 2:3], in1=g2[:], op0=ALU.mult, op1=ALU.mult), reads=[Bx1, Bst, Bwp], writes=[Bhnb])
                psb = psT[:].bitcast(BF16)
                for c in range(8):
                    OP("tensor", lambda e, c=c, hnb=hnb: e.transpose(out=psb[:, c * 128:(c + 1) * 128], in_=hnb[:, c * 128:(c + 1) * 128], identity=ident), reads=[Bhnb], writes=[BpsT])
                OP("vector", lambda e: e.tensor_copy(out=hnT[:], in_=psb.rearrange("p (c t) -> p c t", c=8)), reads=[BpsT], writes=[BhnT])
                for q4 in range(4):
                    pq, Bpq = pq_r.next()
                    for j in range(4):
                        hp = q4 * 4 + j
                        for c in range(8):
                            OP("tensor", lambda e, pq=pq, j=j, hp=hp, c=c: e.matmul(pq[:, j * 128:(j + 1) * 128], lhsT=wpq[:, c, hp * 128:(hp + 1) * 128], rhs=hnT[:, c, :], start=(c == 0), stop=(c == 7)), reads=[Bwp, BhnT], writes=[Bpq])
                    OP(*cp(qpT[:, q4 * 4:(q4 + 1) * 4, :], pq[:].rearrange("p (j t) -> p j t", j=4)), reads=[Bpq], writes=[BqpT])
                for q4 in range(4):
                    pq, Bpq = pq_r.next()
                    for j in range(4):
                        hp = q4 * 4 + j
                        OP("tensor", lambda e, pq=pq, j=j, hp=hp: e.matmul(pq[:, j * 128:(j + 1) * 128], lhsT=qpT[:, hp, :], rhs=skT[:, hp, :], start=True, stop=True), reads=[Bwp, BqpT], writes=[Bpq])
                    OP(*cp(s_sb[:, q4 * 4:(q4 + 1) * 4, :], pq[:].rearrange("p (j t) -> p j t", j=4)), reads=[Bpq], writes=[Bs])
                mark = len(R)
                for hp in range(16):
                    OP("vector", lambda e, hp=hp: e.max(out=vals[:, hp, 0:8], in_=s_sb[:, hp, :]), reads=[Bs], writes=[Bvals])
                    OP("vector", lambda e, hp=hp: e.match_replace(out=tmpS[:, hp, :], in_to_replace=vals[:, hp, 0:8], in_values=s_sb[:, hp, :], imm_value=-1e30), reads=[Bs, Bvals], writes=[Btmp])
                    OP("vector", lambda e, hp=hp: e.max(out=vals[:, hp, 8:16], in_=tmpS[:, hp, :]), reads=[Btmp], writes=[Bvals])
                    OP("vector", lambda e, hp=hp: e.max_index(out=idxu[:, hp, 0:8], in_max=vals[:, hp, 0:8], in_values=s_sb[:, hp, :]), reads=[Bs, Bvals], writes=[Bidx])
                    OP("vector", lambda e, hp=hp: e.max_index(out=idxu[:, hp, 8:16], in_max=vals[:, hp, 8:16], in_values=tmpS[:, hp, :]), reads=[Btmp, Bvals], writes=[Bidx])
                vv4 = vals[:].rearrange("p (h two) k -> p h two k", two=2)
                cand4 = cand.rearrange("p h (i j) -> p h i j", j=16)
                OP("vector", lambda e: e.tensor_tensor(out=cand4, in0=vv4[:, :, 0, :].unsqueeze(3).broadcast_to([128, 8, 16, 16]), in1=vv4[:, :, 1, :].unsqueeze(2).broadcast_to([128, 8, 16, 16]), op=ALU.add), reads=[Bvals], writes=[Bcand])
                for h in range(8):
                    OP("vector", lambda e, h=h: e.max(out=best[:, h, 0:8], in_=cand[:, h, :]), reads=[Bcand], writes=[Bbest])
                    OP("vector", lambda e, h=h: e.match_replace(out=ctmp[:, h, :], in_to_replace=best[:, h, 0:8], in_values=cand[:, h, :], imm_value=-1e30), reads=[Bcand, Bbest], writes=[Bctmp])
                    OP("vector", lambda e, h=h: e.max(out=best[:, h, 8:16], in_=ctmp[:, h, :]), reads=[Bctmp], writes=[Bbest])
                    OP("vector", lambda e, h=h: e.max_index(out=ciu[:, h, 0:8], in_max=best[:, h, 0:8], in_values=cand[:, h, :]), reads=[Bcand, Bbest], writes=[Bci])
                    OP("vector", lambda e, h=h: e.max_index(out=ciu[:, h, 8:16], in_max=best[:, h, 8:16], in_values=ctmp[:, h, :]), reads=[Bctmp, Bbest], writes=[Bci])
                OP("vector", lambda e: e.tensor_scalar(out=hiu[:], in0=ciu[:], scalar1=4, scalar2=None, op0=ALU.logical_shift_right), reads=[Bci], writes=[Bhl])
                OP("vector", lambda e: e.tensor_scalar(out=lou[:], in0=ciu[:], scalar1=15, scalar2=None, op0=ALU.bitwise_and), reads=[Bci], writes=[Bhl])
                OP("vector", lambda e: e.tensor_copy(out=hif[:], in_=hiu[:]), reads=[Bhl], writes=[Bhl])
                OP("vector", lambda e: e.tensor_copy(out=lof[:], in_=lou[:]), reads=[Bhl], writes=[Bhl])
                OP("vector", lambda e: e.tensor_copy(out=idxf[:], in_=idxu[:]), reads=[Bidx], writes=[Bidxf])
                if4 = idxf[:].rearrange("p (h two) k -> p h two k", two=2)
                oh4 = ctmp.rearrange("p h (k i) -> p h k i", i=16)
                iota_b = iota_f[:].unsqueeze(1).unsqueeze(1).broadcast_to([128, 8, 16, 16])
                for which, srcf, half in (("hi", hif, 0), ("lo", lof, 1)):
                    OP("vector", lambda e, srcf=srcf: e.tensor_tensor(out=oh4, in0=srcf[:].unsqueeze(3).broadcast_to([128, 8, 16, 16]), in1=iota_b, op=ALU.is_equal), reads=[Bhl, Bwp, Bctmp, Bci], writes=[Bctmp])
                    OP("vector", lambda e, half=half: e.tensor_tensor(out=oh4, in0=oh4, in1=if4[:, :, half, :].unsqueeze(2).broadcast_to([128, 8, 16, 16]), op=ALU.mult), reads=[Bctmp, Bidxf], writes=[Bctmp])
                    OP("vector", lambda e, half=half: e.tensor_reduce(out=i12[:, half, :], in_=ctmp.rearrange("p h (k i) -> p (h k) i", i=16), axis=AX.X, op=ALU.add), reads=[Bctmp], writes=[Bi12])
                OP("vector", lambda e: e.scalar_tensor_tensor(out=ef[:], in0=i12[:, 0, :], scalar=128.0, in1=i12[:, 1, :], op0=ALU.mult, op1=ALU.add), reads=[Bi12], writes=[Bef])
                OP("vector", lambda e, eidx=eidx: e.tensor_copy(out=eidx[:], in_=ef[:]), reads=[Bef], writes=[Beidx])
                OP("vector", lambda e: e.tensor_tensor(out=gtmp[:], in0=best[:], in1=best[:, :, 0:1].broadcast_to([128, 8, 16]), op=ALU.subtract), reads=[Bbest], writes=[Bgt])
                OP("scalar", lambda e: e.activation(out=gtmp[:], in_=gtmp[:], func=AF.Exp), reads=[Bgt], writes=[Bgt])
                OP("vector", lambda e: e.tensor_reduce(out=gsum[:, 0:8], in_=gtmp[:], axis=AX.X, op=ALU.add), reads=[Bgt], writes=[Bgs])
                OP("vector", lambda e: e.reciprocal(out=gsum[:, 8:16], in_=gsum[:, 0:8]), reads=[Bgs], writes=[Bgs])
                OP("vector", lambda e, gate=gate: e.tensor_tensor(out=gate[:].rearrange("p (h k) -> p h k", k=16), in0=gtmp[:], in1=gsum[:, 8:16].unsqueeze(2).broadcast_to([128, 8, 16]), op=ALU.mult), reads=[Bgt, Bgs], writes=[Bgate])

                ctx = dict(sq=sq, rows=rows, x1t=x1t, Bx1=Bx1, hnf=hnf, Bhnf=Bhnf, hnb=hnb, Bhnb=Bhnb, eidx=eidx, Beidx=Beidx, gate=gate, Bgate=Bgate, acol=acol, gacol=gacol)
                ctx["mark"] = mark
                return ctx, R

            ctx, R = make_sel(0)
            for _, th in R:
                th()
            for ti in range(ntiles):
                nctx, nR = make_sel(ti + 1) if ti + 1 < ntiles else (None, [])
                nk = 0
                sq, rows, x1t, Bx1, hnf, Bhnf = ctx["sq"], ctx["rows"], ctx["x1t"], ctx["Bx1"], ctx["hnf"], ctx["Bhnf"]
                hnb, Bhnb = ctx["hnb"], ctx["Bhnb"]
                eidx, Beidx, gate, Bgate, acol, gacol = ctx["eidx"], ctx["Beidx"], ctx["gate"], ctx["Bgate"], ctx["acol"], ctx["gacol"]
                Ba = [Buf() for _ in range(128)]
                Bga = [Buf() for _ in range(128)]
                items = [dict(hk=hk) for hk in range(128)]

                def itA(I):
                    hk = I["hk"]
                    gb, Bgb = gb_r.next()
                    I["gb"], I["Bgb"] = gb, Bgb
                    S.dma("gpsimd", lambda e, gb=gb, hk=hk, eidx=eidx: e.indirect_dma_start(out=gb[:], out_offset=None, in_=uvb, in_offset=bass.IndirectOffsetOnAxis(ap=eidx[:, hk:hk + 1], axis=0)), reads=[Beidx, B_uvb], writes=[Bgb])
                    I["fused"] = (hk % STT_EVERY == STT_EVERY - 1)
                    if I["fused"]:
                        S.op("vector", lambda e, gb=gb, hk=hk, hnb=hnb, acol=acol: e.scalar_tensor_tensor(out=junk[:], in0=gb[:, 0:D], scalar=1.0, in1=hnb[:], op0=ALU.mult, op1=ALU.mult, accum_out=acol[:, hk:hk + 1]), reads=[Bgb, Bhnb], writes=[Ba[hk]])
                        return
                    pd, Bpd = prod_r.next()
                    I["pd"], I["Bpd"] = pd, Bpd
                    S.op("vector", lambda e, gb=gb, pd=pd, hnb=hnb: e.tensor_tensor(out=pd[:], in0=gb[:, 0:D], in1=hnb[:], op=ALU.mult), reads=[Bgb, Bhnb], writes=[Bpd])

                def itA2(I):
                    hk = I["hk"]
                    if I["fused"]:
                        return
                    pd, Bpd = I["pd"], I["Bpd"]
                    S.op("scalar", lambda e, pd=pd, hk=hk, acol=acol: e.activation(out=junkA[:], in_=pd[:], func=AF.Copy, accum_out=acol[:, hk:hk + 1]), reads=[Bpd], writes=[Ba[hk]])

                def itB(I):
                    hk = I["hk"]
                    S.op("scalar", lambda e, hk=hk, acol=acol, gacol=gacol: e.activation(out=gacol[:, hk:hk + 1], in_=acol[:, hk:hk + 1], func=AF.Gelu), reads=[Ba[hk]], writes=[Bga[hk]])

                def itC(I):
                    hk = I["hk"]
                    gb, Bgb = I["gb"], I["Bgb"]
                    dg, Bdg = dg_r.next()
                    S.op(debug.get("dg_eng", "vector"), lambda e, dg=dg, hk=hk, gacol=gacol, gate=gate: e.tensor_scalar(out=dg[:], in0=ident, scalar1=gacol[:, hk:hk + 1], scalar2=gate[:, hk:hk + 1], op0=ALU.mult, op1=ALU.mult), reads=[Bga[hk], Bgate], writes=[Bdg])
                    S.op("tensor", lambda e, dg=dg, gb=gb, hk=hk: e.matmul(acc0[:], lhsT=dg[:], rhs=gb[:, D:D + 512], start=(hk == 0), stop=(hk == 127)), reads=[Bdg, Bgb], writes=[Bacc])
                    S.op("tensor", lambda e, dg=dg, gb=gb, hk=hk: e.matmul(acc1[:], lhsT=dg[:], rhs=gb[:, D + 512:D + 1024], start=(hk == 0), stop=(hk == 127)), reads=[Bdg, Bgb], writes=[Bacc])

                LAG_A2, LAG_B, LAG_C = debug.get("lags", (1, 2, 4))
                for t in range(128 + LAG_C):
                    if t < 128:
                        itA(items[t])
                    if 0 <= t - LAG_A2 < 128:
                        itA2(items[t - LAG_A2])
                    if 0 <= t - LAG_B < 128:
                        itB(items[t - LAG_B])
                    if 0 <= t - LAG_C < 128:
                        itC(items[t - LAG_C])
                    if nctx is not None:
                        nmark = nctx["mark"]
                        if nk < nmark:
                            ne, last_eng = 0, None
                            while nk < nmark and ne < 40:
                                eng_k, th = nR[nk]
                                if last_eng is not None and eng_k != last_eng:
                                    break
                                th()
                                last_eng = eng_k
                                nk += 1
                                ne += 1
                        else:
                            left_steps = max(1, 123 - t)
                            quota = -(-(len(nR) - nk) // left_steps)
                            for _ in range(quota):
                                if nk < len(nR):
                                    nR[nk][1]()
                                    nk += 1
                S.op("vector", lambda e, x1t=x1t: e.tensor_tensor(out=x1t[:, 0:512], in0=acc0[:], in1=x1t[:, 0:512], op=ALU.add), reads=[Bacc, Bx1], writes=[Bx1])
                S.op("vector", lambda e, x1t=x1t: e.tensor_tensor(out=x1t[:, 512:1024], in0=acc1[:], in1=x1t[:, 512:1024], op=ALU.add), reads=[Bacc, Bx1], writes=[Bx1])
                S.dma("sync", lambda e, x1t=x1t, sq=sq, rows=rows: e.dma_start(out=out_d[sq, rows, :], in_=x1t[:]), reads=[Bx1], sembuf=Bx1)
                ctx = nctx
            S.emit()
        print("total ops", S.total_ops, "sems", S.nsem)
    return nc


def prep_inputs(inputs):
    f = lambda a: np.ascontiguousarray(np.asarray(a, dtype=np.float32))
    x = f(inputs["x"])
    shared = {
        "g1": f(np.broadcast_to(np.asarray(inputs["norm1_gain"], np.float32)[0][None, :], (128, D))),
        "w_in": f(inputs["w_in"][0]),
        "b_gate": f(np.asarray(inputs["b_gate"], np.float32)[0].reshape(16, 128).T),
        "qg": f(np.asarray(inputs["q_norm_gain"], np.float32)[0].reshape(6, 128).T),
        "kg": f(np.asarray(inputs["k_norm_gain"], np.float32)[0].reshape(6, 128).T),
        "w_sb_out": f(inputs["w_sb_out"][0]),
        "w_dil_out": f(inputs["w_dil_out"][0]),
        "w_out": f(inputs["w_out"][0]),
        "g2": f(np.broadcast_to(np.asarray(inputs["norm2_gain"], np.float32)[0][None, :], (128, D))),
        "w_peer_q": f(inputs["w_peer_q"][0]),
        "skT": f(np.asarray(inputs["peer_sub_keys"], np.float32)[0].reshape(16, 128, 128).transpose(2, 0, 1)),
        "uv": f(np.concatenate([np.asarray(inputs["peer_u"], np.float32)[0], np.asarray(inputs["peer_v"], np.float32)[0]], axis=1)),
        "consts": host_consts(),
        "alibi": host_alibi(),
    }
    in_maps = []
    for c in range(NCORES):
        m = dict(shared)
        m["x"] = np.ascontiguousarray(x[c * NSEQ:(c + 1) * NSEQ])
        in_maps.append(m)
    return in_maps


def kernel(**inputs):
    in_maps = prep_inputs(inputs)
    nc = build()
    res = run_bass_kernel_spmd(nc, in_maps, core_ids=list(range(NCORES)))
    return np.concatenate([r["out"] for r in res.results], axis=0)
```
